# Optimizing a Trainium2 kernel written in Bass

```python
import jax, jax.numpy as jnp
from jax import lax
import numpy as np

D_MODEL = 1024
BATCH = 1
SEQ = 16384
DEPTH = 1

CHUNK = 64
N_META = 16
CONV_CH = 512
CONV_WIDTH = 31
SB_HEADS = 8
SB_HEAD_DIM = 64
SB_WIDTH = SB_HEADS * SB_HEAD_DIM
Q_BLOCK = 128
IN_COLS = 2 * CONV_CH + 3 * SB_WIDTH
PEER_HEADS = 8
PEER_QDIM = 256
PEER_QHALF = PEER_QDIM // 2
N_KEYS = 128
N_EXPERTS = N_KEYS * N_KEYS
PEER_TOPK = 16
PEER_BLOCK = 128
EPS = 1e-6

kernel_name = "hybrid_conv_stickbreak_peer_block"


def rms_norm(x, g):
    xf = x.astype(jnp.float32)
    y = xf * lax.rsqrt(jnp.mean(xf * xf, axis=-1, keepdims=True) + EPS)
    return (y * g.astype(jnp.float32)).astype(x.dtype)


def layer_norm(x, g, b):
    xf = x.astype(jnp.float32)
    mu = jnp.mean(xf, axis=-1, keepdims=True)
    xc = xf - mu
    y = xc * lax.rsqrt(jnp.mean(xc * xc, axis=-1, keepdims=True) + EPS)
    return (y * g.astype(jnp.float32) + b.astype(jnp.float32)).astype(x.dtype)


def conv_module(a, gt, w_dw, b_dw, ln_g, ln_b, w_out):
    h = a * jax.nn.sigmoid(gt)
    h = lax.conv_general_dilated(
        h, w_dw[:, None, :], window_strides=(1,),
        padding=[(CONV_WIDTH - 1, 0)],
        dimension_numbers=('NWC', 'WIO', 'NWC'),
        feature_group_count=CONV_CH) + b_dw
    h = layer_norm(h, ln_g, ln_b)
    h = jax.nn.silu(h)
    return h @ w_out


def stick_breaking_attention(q, k, v):
    B, H, Lp, dh = q.shape
    nb = Lp // Q_BLOCK
    scale = dh ** -0.5
    kf = k.astype(jnp.float32)
    vf = v.astype(jnp.float32)
    key_pos = jnp.arange(Lp)
    q_blocks = q.reshape(B, H, nb, Q_BLOCK, dh).transpose(2, 0, 1, 3, 4)
    q_pos = jnp.arange(Lp).reshape(nb, Q_BLOCK)

    def block(args):
        q_blk, t = args
        z = jnp.einsum('bhqd,bhkd->bhqk', q_blk.astype(jnp.float32), kf) * scale
        before = key_pos[None, :] < t[:, None]
        log_keep = jnp.where(before, jax.nn.log_sigmoid(-z), 0.0)
        log_rest = lax.cumsum(log_keep, axis=3, reverse=True) - log_keep
        w = jnp.where(before, jnp.exp(jax.nn.log_sigmoid(z) + log_rest), 0.0)
        return jnp.einsum('bhqk,bhkd->bhqd', w, vf)

    o = lax.map(block, (q_blocks, q_pos))
    return o.transpose(1, 2, 0, 3, 4).reshape(B, H, Lp, dh).astype(v.dtype)


def peer_ffn(h, w_q, k1, k2, u, v):
    B, Lp, D = h.shape
    q = (h @ w_q).reshape(B, Lp, PEER_HEADS, 2, PEER_QHALF)
    s1 = jnp.einsum('blhd,hnd->blhn', q[..., 0, :], k1).astype(jnp.float32)
    s2 = jnp.einsum('blhd,hnd->blhn', q[..., 1, :], k2).astype(jnp.float32)
    v1, i1 = lax.top_k(s1, PEER_TOPK)
    v2, i2 = lax.top_k(s2, PEER_TOPK)
    cand = (v1[..., :, None] + v2[..., None, :]).reshape(B, Lp, PEER_HEADS, PEER_TOPK * PEER_TOPK)
    cand_idx = (i1[..., :, None] * N_KEYS + i2[..., None, :]).reshape(B, Lp, PEER_HEADS, PEER_TOPK * PEER_TOPK)
    best, pos = lax.top_k(cand, PEER_TOPK)
    idx = jnp.take_along_axis(cand_idx, pos, axis=-1)
    gate = jax.nn.softmax(best, axis=-1)

    nt = (B * Lp) // PEER_BLOCK
    xs = (h.reshape(nt, PEER_BLOCK, D),
          idx.reshape(nt, PEER_BLOCK, PEER_HEADS, PEER_TOPK),
          gate.reshape(nt, PEER_BLOCK, PEER_HEADS, PEER_TOPK))

    def block(args):
        x_blk, e_blk, g_blk = args
        act = jax.nn.gelu(jnp.einsum('td,thkd->thk', x_blk, u[e_blk]), approximate=False)
        coef = (g_blk * act.astype(jnp.float32)).astype(v.dtype)
        return jnp.einsum('thk,thkd->td', coef, v[e_blk])

    return lax.map(block, xs).reshape(B, Lp, D)


def setup_inputs(seed: int = 0) -> dict:
    key = jax.random.key(seed)
    ks = jax.random.split(key, 24)
    f = jnp.float32
    D = D_MODEL
    nrm = lambda k, shape, s: jax.random.normal(k, shape, f) * s
    return {
        "x": nrm(ks[0], (BATCH, SEQ, D), 1.0),
        "meta_tokens": nrm(ks[1], (N_META, D), 1.0),
        "norm1_g": 1.0 + nrm(ks[2], (DEPTH, D), 0.02),
        "w_in": nrm(ks[3], (DEPTH, D, IN_COLS), D ** -0.5),
        "conv_w": nrm(ks[4], (DEPTH, CONV_WIDTH, CONV_CH), CONV_WIDTH ** -0.5),
        "conv_b": nrm(ks[5], (DEPTH, CONV_CH), 0.02),
        "conv_ln_g": 1.0 + nrm(ks[6], (DEPTH, CONV_CH), 0.02),
        "conv_ln_b": nrm(ks[7], (DEPTH, CONV_CH), 0.02),
        "w_conv_out": nrm(ks[8], (DEPTH, CONV_CH, D), CONV_CH ** -0.5),
        "q_norm_g": 1.0 + nrm(ks[9], (DEPTH, SB_HEAD_DIM), 0.02),
        "k_norm_g": 1.0 + nrm(ks[10], (DEPTH, SB_HEAD_DIM), 0.02),
        "w_sb_out": nrm(ks[11], (DEPTH, SB_WIDTH, D), SB_WIDTH ** -0.5),
        "w_gate": nrm(ks[12], (DEPTH, D, 2 * D), D ** -0.5),
        "b_gate": nrm(ks[13], (DEPTH, 2 * D), 0.02),
        "w_o": nrm(ks[14], (DEPTH, D, D), D ** -0.5),
        "norm2_g": 1.0 + nrm(ks[15], (DEPTH, D), 0.02),
        "peer_wq": nrm(ks[16], (DEPTH, D, PEER_HEADS * PEER_QDIM), D ** -0.5),
        "peer_k1": nrm(ks[17], (DEPTH, PEER_HEADS, N_KEYS, PEER_QHALF), PEER_QHALF ** -0.5),
        "peer_k2": nrm(ks[18], (DEPTH, PEER_HEADS, N_KEYS, PEER_QHALF), PEER_QHALF ** -0.5),
        "peer_u": nrm(ks[19], (DEPTH, N_EXPERTS, D), D ** -0.5),
        "peer_v": nrm(ks[20], (DEPTH, N_EXPERTS, D), PEER_HEADS ** -0.5),
    }


def reference(x, meta_tokens, norm1_g, w_in, conv_w, conv_b, conv_ln_g, conv_ln_b, w_conv_out,
              q_norm_g, k_norm_g, w_sb_out, w_gate, b_gate, w_o, norm2_g,
              peer_wq, peer_k1, peer_k2, peer_u, peer_v):
    B, S, D = x.shape
    L = N_META + S
    Lp = -(-L // Q_BLOCK) * Q_BLOCK
    meta = jnp.broadcast_to(meta_tokens[None].astype(x.dtype), (B, N_META, D))
    h = jnp.concatenate([meta, x, jnp.zeros((B, Lp - L, D), x.dtype)], axis=1)
    splits = [CONV_CH, 2 * CONV_CH, 2 * CONV_CH + SB_WIDTH, 2 * CONV_CH + 2 * SB_WIDTH]

    for l in range(DEPTH):
        hn = rms_norm(h, norm1_g[l])
        a, gt, q, k, v = jnp.split(hn @ w_in[l], splits, axis=-1)

        y_conv = conv_module(a, gt, conv_w[l], conv_b[l], conv_ln_g[l], conv_ln_b[l], w_conv_out[l])

        q = rms_norm(q.reshape(B, Lp, SB_HEADS, SB_HEAD_DIM), q_norm_g[l]).transpose(0, 2, 1, 3)
        k = rms_norm(k.reshape(B, Lp, SB_HEADS, SB_HEAD_DIM), k_norm_g[l]).transpose(0, 2, 1, 3)
        v = v.reshape(B, Lp, SB_HEADS, SB_HEAD_DIM).transpose(0, 2, 1, 3)
        o = stick_breaking_attention(q, k, v).transpose(0, 2, 1, 3).reshape(B, Lp, SB_WIDTH)
        y_sb = o @ w_sb_out[l]

        g_conv, g_sb = jnp.split(jax.nn.sigmoid(hn @ w_gate[l] + b_gate[l]), 2, axis=-1)
        h = h + (g_conv * y_conv + g_sb * y_sb) @ w_o[l]

        h = h + peer_ffn(rms_norm(h, norm2_g[l]), peer_wq[l], peer_k1[l], peer_k2[l],
                         peer_u[l], peer_v[l])

    return h[:, N_META:N_META + S]
```

```python
import numpy as np
import ml_dtypes
from contextlib import ExitStack

import concourse.bass as bass
import concourse.mybir as mybir
from concourse.bass_utils import run_bass_kernel_spmd

F32 = mybir.dt.float32
BF16 = mybir.dt.bfloat16
AF = mybir.ActivationFunctionType
ALU = mybir.AluOpType
AX = mybir.AxisListType

NCORE = 8
D = 1024
SEQ = 16384
NMETA = 16
NKB = 129
LKP = NKB * 128
NSLOT = 4
SLOT = 512
HALO = 32
NBLK = 16
EPS = 1e-6
CH = 8192
ACT_HEADS = (1, 4, 7)
HEAD_ORDER = (0, 2, 1, 3, 5, 4, 6, 7)
N_WARM1 = 0
N_WARM = 0
NEG = -1.0e30

C_G1, C_G2, C_CW, C_CB, C_LG, C_LB, C_BG, C_GQ, C_GK, C_END = 0, 8, 16, 140, 144, 148, 152, 168, 169, 170


class _Rec:
    def __init__(self):
        self.call = None

    def __getattr__(self, name):
        def f(*a, **k):
            self.call = (name, a, k)
            return self
        return f


class Prog:
    ENG = ['pe', 'act', 'dve', 'pool', 'sp']

    def __init__(self, nc, stack, n_dma_sems=12):
        self.nc = nc
        self.stack = stack
        self.q = {e: [] for e in self.ENG}
        self.seq = {e: 0 for e in self.ENG}
        self.sems = {e: [] for e in self.ENG}
        self.waited = {e: {} for e in self.ENG}
        self.lastw = {}
        self.readers = {}
        self.ndma = n_dma_sems
        self.dma_sems = [stack.enter_context(nc.semaphore(f"dma{i}")) for i in range(n_dma_sems)]
        self.dma_count = 0
        self.dma_waited_set = {e: set() for e in self.ENG}
        self.nwait = 0

    def _sem(self, e, seq):
        i = (seq - 1) // CH
        while len(self.sems[e]) <= i:
            self.sems[e].append(self.stack.enter_context(self.nc.semaphore(f"s_{e}_{len(self.sems[e])}")))
        return self.sems[e][i], ((seq - 1) % CH) + 1

    def _need(self, e, dep):
        if dep[0] == 'dma':
            idx = dep[1]
            if idx in self.dma_waited_set[e]:
                return
            self.dma_waited_set[e].add(idx)
            sem = self.dma_sems[idx % self.ndma]
            val = 16 * (idx // self.ndma + 1)
            self.q[e].append(('wait', sem, val))
            self.nwait += 1
        else:
            _, pe_, seq = dep
            if self.waited[e].get(pe_, 0) >= seq:
                return
            self.waited[e][pe_] = seq
            sem, val = self._sem(pe_, seq)
            self.q[e].append(('wait', sem, val))
            self.nwait += 1

    def _deps(self, e, reads, writes):
        deps = []
        for r in reads:
            if r in self.lastw:
                deps.append(self.lastw[r])
        for w in writes:
            if w in self.lastw:
                d = self.lastw[w]
                if not (e == 'pe' and d[0] == 'eng' and d[1] == 'pe'):
                    deps.append(d)
            for d in self.readers.get(w, {}).values():
                if d[0] == 'eng' and d[1] == e and e == 'pe':
                    continue
                deps.append(d)
        return deps

    def op(self, e, fn, reads=(), writes=()):
        for d in self._deps(e, reads, writes):
            self._need(e, d)
        self.seq[e] += 1
        s = self.seq[e]
        sem, _ = self._sem(e, s)
        rec = _Rec()
        fn(rec)
        assert rec.call is not None
        self.q[e].append(('op', rec.call, sem))
        me = ('eng', e, s)
        for w in writes:
            self.lastw[w] = me
            self.readers[w] = {}
        for r in reads:
            if r not in writes:
                self.readers.setdefault(r, {})[e] = me
        return me

    def dma(self, out, in_, reads=(), writes=(), **kw):
        e = 'sp'
        idx = self.dma_count
        self.dma_count += 1
        if idx - self.ndma >= 0:
            self._need(e, ('dma', idx - self.ndma))
        for d in self._deps(e, reads, writes):
            self._need(e, d)
        sem = self.dma_sems[idx % self.ndma]
        self.q[e].append(('dma', out, in_, sem, kw))
        me = ('dma', idx)
        for w in writes:
            self.lastw[w] = me
            self.readers[w] = {}
        for r in reads:
            self.readers.setdefault(r, {})[('dma', idx)] = me
        return me

    def barrier(self):
        for e in self.ENG:
            for p in self.ENG:
                if p != e and self.seq[p] > 0:
                    self._need(e, ('eng', p, self.seq[p]))
            for idx in range(max(0, self.dma_count - self.ndma), self.dma_count):
                self._need(e, ('dma', idx))
        self.lastw = {}
        self.readers = {}

    def wait_all_dma(self, e='sp'):
        for idx in range(max(0, self.dma_count - self.ndma), self.dma_count):
            self._need(e, ('dma', idx))

    def flush(self):
        nc = self.nc
        q = self.q
        self.q = {e: [] for e in self.ENG}

        def run(eng, items):
            for it in items:
                if it[0] == 'wait':
                    eng.wait_ge(it[1], it[2])
                elif it[0] == 'op':
                    name, a, k = it[1]
                    getattr(eng, name)(*a, **k).then_inc(it[2], 1)
                else:
                    eng.dma_start(out=it[1], in_=it[2], **it[4]).then_inc(it[3], 16)

        with nc.Block() as block:
            @block.tensor
            def _(eng):
                run(eng, q['pe'])

            @block.scalar
            def _(eng):
                run(eng, q['act'])

            @block.vector
            def _(eng):
                run(eng, q['dve'])

            @block.gpsimd
            def _(eng):
                run(eng, q['pool'])

            @block.sync
            def _(eng):
                run(eng, q['sp'])


def build_program(stop_after=99, att_pairs=4, att_slots=4, dbg=None, p4_stop=99, skip_kv=False):
    nc = bass.Bass("TRN2", target_bir_lowering=False)

    def din(name, shape, dt=F32):
        return nc.dram_tensor(name, list(shape), dt, kind="ExternalInput").ap()

    hall = din("hall", [130 * 128, D])
    xq = din("xq", [NSLOT, SLOT + HALO, D])
    amask_d = din("amask", [128, 33 * 512], BF16)
    cst_d = din("cst", [128, 5 * 128], BF16)
    spk_d = din("spk", [128, C_END])
    w_in = din("w_in", [D, 2560])
    w_conv_out = din("w_conv_out", [512, D])
    w_sb_out = din("w_sb_out", [512, D])
    w_gate = din("w_gate", [D, 2048])
    w_o = din("w_o", [D, D])
    peer_wq = din("peer_wq", [D, 2048])
    peer_k = din("peer_k", [16, 128, 128])
    NE_ = 16384 if stop_after > 4 else 128
    peer_u = din("peer_u", [NE_, D])
    peer_v = din("peer_v", [NE_, D])
    out_d = nc.dram_tensor("out", [NBLK * 128, D], F32, kind="ExternalOutput").ap()
    dbg_d = None
    if dbg is not None:
        dbg_d = nc.dram_tensor("dbg", list(dbg), F32, kind="ExternalOutput").ap()

    kT_scr = nc.dram_tensor("kT_scr", [4, 128, LKP], BF16, kind="Internal").ap()
    v_scr = nc.dram_tensor("v_scr", [4, 128, NKB, 128], BF16, kind="Internal").ap()
    h1_scr = nc.dram_tensor("h1_scr", [NBLK * 128, D], F32, kind="Internal").ap()
    ct_scr = nc.dram_tensor("ct_scr", [128, 128, NBLK * 128], BF16, kind="Internal").ap()

    with ExitStack() as glob:
        P = Prog(nc, glob)

        def GT(name, shape, dt):
            return glob.enter_context(nc.sbuf_tensor("g_" + name, list(shape), dt))

        cst = GT("cst", [128, 640], BF16)
        spk = GT("spk", [128, C_END], F32)
        ident = cst[:, 0:128]
        NTi = cst[:, 128:256]
        NTl = cst[:, 256:384]
        onesm = cst[:, 384:512]
        NOnes = cst[:, 512:640]
        P.dma(cst[:], cst_d, writes=['cst'])
        P.dma(spk[:], spk_d, writes=['spk'])
        g1t = spk[:, C_G1:C_G1 + 8]
        g2t = spk[:, C_G2:C_G2 + 8]

        def rms_rows(ph, xin_ap, R, xn_ap, sst, xin_res, xn_res, junk, sst_res):
            P.op('act', lambda e: e.activation(out=junk[:R, :], in_=xin_ap, func=AF.Square,
                                               accum_out=sst[:R, 0:1]),
                 reads=[xin_res], writes=['junk', sst_res])
            P.op('act', lambda e: e.activation(out=sst[:R, 1:2], in_=sst[:R, 0:1], func=AF.Ln,
                                               bias=EPS, scale=1.0 / D),
                 reads=[sst_res], writes=[sst_res])
            P.op('act', lambda e: e.activation(out=sst[:R, 2:3], in_=sst[:R, 1:2], func=AF.Exp, scale=-0.5),
                 reads=[sst_res], writes=[sst_res])
            P.op('dve', lambda e: e.tensor_scalar(out=xn_ap, in0=xin_ap, scalar1=sst[:R, 2:3], scalar2=None,
                                                  op0=ALU.mult),
                 reads=[xin_res, sst_res], writes=[xn_res])

        class WPool:
            def __init__(self, stack, nbuf, nstage=2, tag="w"):
                self.wb = [stack.enter_context(nc.sbuf_tensor(f"{tag}b{i}", [128, 8, 512], BF16)) for i in range(nbuf)]
                self.ws = [stack.enter_context(nc.sbuf_tensor(f"{tag}s{i}", [128, 8, 512], F32)) for i in range(nstage)]
                self.tag = tag
                self.i = 0
                self.k = 0

            def load(self, src2d, kc=8, ncols=512, scale=None, kp=128):
                b = self.i % len(self.wb)
                self.i += 1
                s = self.k % len(self.ws)
                self.k += 1
                wb, ws = self.wb[b], self.ws[s]
                P.dma(ws[0:kp, 0:kc, 0:ncols], src2d.rearrange("(c p) n -> p c n", p=kp),
                      writes=[f'{self.tag}s{s}'])
                if scale is None:
                    P.op('pool', lambda e: e.tensor_copy(out=wb[0:kp, 0:kc, 0:ncols], in_=ws[0:kp, 0:kc, 0:ncols]),
                         reads=[f'{self.tag}s{s}'], writes=[f'{self.tag}b{b}'])
                else:
                    P.op('pool', lambda e: e.tensor_tensor(out=wb[0:kp, 0:kc, 0:ncols], in0=ws[0:kp, 0:kc, 0:ncols],
                                                           in1=scale.unsqueeze(2).to_broadcast([kp, kc, ncols]),
                                                           op=ALU.mult),
                         reads=[f'{self.tag}s{s}', 'spk'], writes=[f'{self.tag}b{b}'])
                return wb, f'{self.tag}b{b}'

        def dbg_dump(src_ap, dst_ap, res):
            P.dma(dst_ap, src_ap, reads=[res], writes=['dbg'])

        s_o = ExitStack()
        oT_all = s_o.enter_context(nc.sbuf_tensor("g_oT_all", [64, 8, NBLK * 128], BF16))
        s_q = ExitStack()
        qT_all = s_q.enter_context(nc.sbuf_tensor("g_qT_all", [128, 4, NBLK * 128], BF16))
        with ExitStack() as ph:
            def T(name, shape, dt):
                return ph.enter_context(nc.sbuf_tensor("p0_" + name, list(shape), dt))

            def PS(name, shape, dt):
                return ph.enter_context(nc.psum_tensor("ps0_" + name, list(shape), dt))

            wp = WPool(ph, 3, 2, tag="w1")
            wk, wk_r = wp.load(w_in[:, 1536:2048], scale=g1t)
            wv, wv_r = wp.load(w_in[:, 2048:2560], scale=g1t)
            wq, wq_r = wp.load(w_in[:, 1024:1536], scale=g1t)
            xin = [T(f"xin{i}", [128, D], F32) for i in range(3)]
            junk = T("junk", [128, D], BF16)
            sst = [T(f"sst{i}", [128, 4], F32) for i in range(3)]
            xn = [T(f"xn{i}", [128, D], BF16) for i in range(3)]
            hnT = [T(f"hnT{i}", [128, 8, 128], BF16) for i in range(2)]
            ksq = T("ksq", [128, 512], F32)
            kst = [T(f"kst{i}", [128, 24], F32) for i in range(2)]
            kn = [T(f"kn{i}", [128, 512], BF16) for i in range(2)]
            kTb = [T(f"kTb{i}", [128, 4, 128], BF16) for i in range(2)]
            vb = [T(f"vb{i}", [128, 512], BF16) for i in range(2)]
            ptr = [PS(f"ptr{i}", [128, 1024], BF16) for i in range(2)]
            kps = [PS(f"kps{i}", [128, 512], F32) for i in range(2)]
            vps = [PS(f"vps{i}", [128, 512], F32) for i in range(2)]
            pkt = PS("pkt", [128, 1024], BF16)
            DZ1 = PS("DZ1", [128, 512], F32) if N_WARM1 else None

            def front(i, src_rows, w_t, w_r, do_v, mid=None, stage='ab'):
                b = i % 2
                if 'a' in stage:
                    front_a(b, src_rows, i % 3)
                if 'b' in stage:
                    front_b(b, w_t, w_r, do_v, mid)

            def front_a(b, src_rows, b3):
                P.dma(xin[b3][:], src_rows, writes=[f'xin{b3}'])
                rms_rows(ph, xin[b3][:], 128, xn[b3][:], sst[b3], f'xin{b3}', f'xn{b3}', junk, f'sst{b3}')
                for c in range(8):
                    P.op('pe', lambda e, c=c: e.transpose(out=ptr[b][:, c * 128:(c + 1) * 128],
                                                          in_=xn[b3][:, c * 128:(c + 1) * 128], identity=ident),
                         reads=[f'xn{b3}', 'cst'], writes=[f'ptr{b}'])
                P.op('act', lambda e: e.activation(out=hnT[b][:].rearrange("p c n -> p (c n)"), in_=ptr[b][:],
                                                   func=AF.Copy),
                     reads=[f'ptr{b}'], writes=[f'hnT{b}'])

            def front_b(b, w_t, w_r, do_v, mid):
                if mid is not None:
                    mid()
                for c in range(8):
                    P.op('pe', lambda e, c=c: e.matmul(kps[b][:], lhsT=hnT[b][:, c, :], rhs=w_t[:, c, :],
                                                       start=(c == 0), stop=(c == 7)),
                         reads=[f'hnT{b}', w_r], writes=[f'kps{b}'])
                if do_v:
                    for c in range(8):
                        P.op('pe', lambda e, c=c: e.matmul(vps[b][:], lhsT=hnT[b][:, c, :], rhs=wv[:, c, :],
                                                           start=(c == 0), stop=(c == 7)),
                             reads=[f'hnT{b}', wv_r], writes=[f'vps{b}'])
                for _ in range(N_WARM1):
                    P.op('pe', lambda e: e.matmul(DZ1[:], lhsT=ident, rhs=wk[:, 0, :], start=True, stop=True),
                         reads=[wk_r, 'cst'], writes=['DZ1'])
                P.op('act', lambda e: e.activation(out=ksq[:], in_=kps[b][:], func=AF.Square),
                     reads=[f'kps{b}'], writes=['ksq'])
                P.op('dve', lambda e: e.tensor_reduce(out=kst[b][:, 0:8],
                                                      in_=ksq[:].rearrange("p (h d) -> p h d", h=8),
                                                      axis=AX.X, op=ALU.add),
                     reads=['ksq'], writes=[f'kst{b}'])
                P.op('act', lambda e: e.activation(out=kst[b][:, 8:16], in_=kst[b][:, 0:8], func=AF.Ln,
                                                   bias=EPS, scale=1.0 / 64),
                     reads=[f'kst{b}'], writes=[f'kst{b}'])
                P.op('act', lambda e: e.activation(out=kst[b][:, 16:24], in_=kst[b][:, 8:16], func=AF.Exp, scale=-0.5),
                     reads=[f'kst{b}'], writes=[f'kst{b}'])
                P.op('dve', lambda e: e.tensor_tensor(out=kn[b][:].rearrange("p (h d) -> p h d", h=8),
                                                      in0=kps[b][:].rearrange("p (h d) -> p h d", h=8),
                                                      in1=kst[b][:, 16:24].unsqueeze(2).to_broadcast([128, 8, 64]),
                                                      op=ALU.mult),
                     reads=[f'kps{b}', f'kst{b}'], writes=[f'kn{b}'])
                if do_v:
                    P.op('act', lambda e: e.activation(out=vb[b][:], in_=vps[b][:], func=AF.Copy),
                         reads=[f'vps{b}'], writes=[f'vb{b}'])

            def back(i, is_q, pb):
                b = i % 2
                for pr in range(4):
                    P.op('pe', lambda e, pr=pr: e.transpose(out=pkt[:, pr * 128:(pr + 1) * 128],
                                                            in_=kn[b][:, pr * 128:(pr + 1) * 128], identity=ident),
                         reads=[f'kn{b}', 'cst'], writes=['pkt'])
                if not is_q:
                    P.op('dve', lambda e: e.tensor_scalar(out=kTb[b][:].rearrange("p a n -> p (a n)"),
                                                          in0=pkt[:, 0:512], scalar1=spk[:, C_GK:C_GK + 1],
                                                          scalar2=None, op0=ALU.mult),
                         reads=['pkt', 'spk'], writes=[f'kTb{b}'])
                    P.dma(kT_scr[:, :, pb * 128:(pb + 1) * 128].rearrange("a p n -> p a n"), kTb[b][:],
                          reads=[f'kTb{b}'], writes=['kT_scr'])
                    P.dma(v_scr[:, :, pb, :].rearrange("a p n -> p a n"),
                          vb[b][:].rearrange("p (a n) -> p a n", a=4),
                          reads=[f'vb{b}'], writes=['v_scr'])
                else:
                    P.op('dve', lambda e: e.tensor_scalar(
                        out=qT_all[:, :, pb * 128:(pb + 1) * 128],
                        in0=pkt[:, 0:512].rearrange("p (a n) -> p a n", a=4),
                        scalar1=spk[:, C_GQ:C_GQ + 1], scalar2=0.125, op0=ALU.mult, op1=ALU.mult),
                         reads=['pkt', 'spk'], writes=['qT_all'])

            n1 = 0 if skip_kv else NKB
            items = [(False, pb) for pb in range(n1)] + [(True, blk) for blk in range(NBLK)]
            def item_args(is_q, idx):
                if is_q:
                    j, sb = idx // 4, idx % 4
                    return (xq[j, HALO + sb * 128:HALO + (sb + 1) * 128, :], wq, wq_r, False)
                return (hall[16 + idx * 128:16 + (idx + 1) * 128, :], wk, wk_r, True)

            prev = None
            front(0, *item_args(*items[0]), stage='a')
            for i, (is_q, idx) in enumerate(items):
                if i + 1 < len(items):
                    front(i + 1, *item_args(*items[i + 1]), stage='a')
                mid = (lambda pv=prev: back(*pv)) if prev is not None else None
                front(i, *item_args(is_q, idx), mid=mid, stage='b')
                prev = (i, is_q, idx)
            back(*prev)
            P.barrier()
            P.flush()

        if stop_after <= 1:
            with ExitStack() as ph:
                t = ph.enter_context(nc.sbuf_tensor("d1_dbt", [128, 2048], BF16))
                t32 = ph.enter_context(nc.sbuf_tensor("d1_dbt32", [128, 2048], F32))
                P.dma(t[:], kT_scr[0, :, 0:2048], writes=['dbt'])
                P.op('dve', lambda e: e.tensor_copy(out=t32[:], in_=t[:]), reads=['dbt'], writes=['dbt32'])
                P.dma(dbg_d[0], t32[:], reads=['dbt32'], writes=['dbg'])
                P.dma(t[:].rearrange("p (a n) -> p a n", a=16), v_scr[0, :, 0:16, :], writes=['dbt'])
                P.op('dve', lambda e: e.tensor_copy(out=t32[:], in_=t[:]), reads=['dbt'], writes=['dbt32'])
                P.dma(dbg_d[1], t32[:], reads=['dbt32'], writes=['dbg'])
                P.op('dve', lambda e: e.tensor_copy(out=t32[:], in_=qT_all[:, 0, :]), reads=['dbt', 'qT_all'],
                     writes=['dbt32'])
                P.dma(dbg_d[2], t32[:], reads=['dbt32'], writes=['dbg'])
                P.wait_all_dma()
                P.barrier()
                P.flush()
            s_q.close()
            s_o.close()
            return nc

        with ExitStack() as ph:
            def T(name, shape, dt):
                return ph.enter_context(nc.sbuf_tensor("p1_" + name, list(shape), dt))

            def PS(name, shape, dt):
                return ph.enter_context(nc.psum_tensor("ps1_" + name, list(shape), dt))

            amask = T("amask", [128, 33 * 512], BF16)
            P.dma(amask[:], amask_d, writes=['amask'])
            kTp = T("kTp", [128, LKP], BF16)
            vp = T("vp", [128, NKB, 128], BF16)
            NE1, NSP, NER, NW, NZ = 6, 5, 3, 3, 3
            e1 = [T(f"e1_{i}", [128, 512], F32) for i in range(NE1)]
            spb = [T(f"sp_{i}", [128, 512], BF16) for i in range(NSP)]
            er = [T(f"er_{i}", [128, 512], F32) for i in range(NER)]
            wt = [T(f"wt_{i}", [128, 512], BF16) for i in range(NW)]
            CS = [[T(f"cs{s_}_{i}", [128, 512], BF16) for i in range(2)] for s_ in range(2)]
            Z = [PS(f"Z{i}", [128, 512], F32) for i in range(NZ)]
            RA = [PS(f"RA{i}", [128, 512], F32) for i in range(2)]
            OT = [PS(f"OT{i}", [128, 512], F32) for i in range(2)]
            DZ = PS("DZ", [128, 512], F32) if N_WARM else None

            if att_pairs < 4 or att_slots < 4:
                P.op('dve', lambda e: e.memset(oT_all[:], 0.0), writes=['oT_all'])
            for pr in range(att_pairs):
                for q4 in range(4):
                    c0, c1 = q4 * (LKP // 4), (q4 + 1) * (LKP // 4)
                    P.dma(kTp[:, c0:c1], kT_scr[pr, :, c0:c1], reads=['kT_scr'], writes=['kTp'])
                P.dma(vp[:, 0:64, :], v_scr[pr, :, 0:64, :], reads=['v_scr'], writes=['vp'])
                P.dma(vp[:, 64:NKB, :], v_scr[pr, :, 64:NKB, :], reads=['v_scr'], writes=['vp'])
                for j in range(att_slots):
                    nkb = 32 * j + 33
                    tiles = []
                    for kk in range(nkb):
                        kb = 32 * j + 32 - kk
                        for s in range(2):
                            tiles.append((s, kb, kk))
                    NT = len(tiles)
                    qcols = slice(j * 512, (j + 1) * 512)

                    def QK(t):
                        s, kb, kk = tiles[t]
                        ho = s * 64
                        P.op('pe', lambda e: e.matmul(Z[t % NZ][:], lhsT=kTp[ho:ho + 64, kb * 128:(kb + 1) * 128],
                                                      rhs=qT_all[ho:ho + 64, pr, qcols], start=True, stop=True),
                             reads=['kTp', 'qT_all'], writes=[f'Z{t % NZ}'])

                    def EXP1(t):
                        s, kb, kk = tiles[t]
                        P.op('act', lambda e: e.activation(out=e1[t % NE1][:], in_=Z[t % NZ][:], func=AF.Exp),
                             reads=[f'Z{t % NZ}'], writes=[f'e1_{t % NE1}'])
                        r = kb - 32 * j
                        if 0 <= r <= 32:
                            P.op('dve', lambda e: e.tensor_tensor(out=e1[t % NE1][:], in0=e1[t % NE1][:],
                                                                  in1=amask[:, r * 512:(r + 1) * 512], op=ALU.mult),
                                 reads=[f'e1_{t % NE1}', 'amask'], writes=[f'e1_{t % NE1}'])

                    def LN(t):
                        P.op('act', lambda e: e.activation(out=spb[t % NSP][:], in_=e1[t % NE1][:], func=AF.Ln,
                                                           bias=1.0, scale=1.0),
                             reads=[f'e1_{t % NE1}'], writes=[f'sp_{t % NSP}'])

                    def TRI(t):
                        s, kb, kk = tiles[t]
                        P.op('pe', lambda e: e.matmul(RA[s][:], lhsT=NTi, rhs=spb[t % NSP][:],
                                                      start=True, stop=(kk == 0)),
                             reads=[f'sp_{t % NSP}', 'cst'], writes=[f'RA{s}'])
                        if kk > 0:
                            P.op('pe', lambda e: e.matmul(RA[s][:], lhsT=NOnes, rhs=CS[s][kk % 2][:],
                                                          start=False, stop=True),
                                 reads=[f'cs{s}_{kk % 2}', 'cst'], writes=[f'RA{s}'])
                        if kk < nkb - 1:
                            if kk == 0:
                                P.op('dve', lambda e: e.tensor_copy(out=CS[s][1][:], in_=spb[t % NSP][:]),
                                     reads=[f'sp_{t % NSP}'], writes=[f'cs{s}_1'])
                            else:
                                P.op('dve', lambda e: e.tensor_tensor(out=CS[s][(kk + 1) % 2][:],
                                                                       in0=CS[s][kk % 2][:], in1=spb[t % NSP][:],
                                                                       op=ALU.add),
                                     reads=[f'cs{s}_{kk % 2}', f'sp_{t % NSP}'], writes=[f'cs{s}_{(kk + 1) % 2}'])

                    def EXPR(t):
                        s, kb, kk = tiles[t]
                        P.op('act', lambda e: e.activation(out=er[t % NER][:], in_=RA[s][:], func=AF.Exp),
                             reads=[f'RA{s}'], writes=[f'er_{t % NER}'])

                    def WMUL(t):
                        P.op('dve', lambda e: e.tensor_tensor(out=wt[t % NW][:], in0=e1[t % NE1][:],
                                                              in1=er[t % NER][:], op=ALU.mult),
                             reads=[f'e1_{t % NE1}', f'er_{t % NER}'], writes=[f'wt_{t % NW}'])

                    def AV(t):
                        s, kb, kk = tiles[t]
                        ho = s * 64
                        P.op('pe', lambda e: e.matmul(OT[s][0:64, :], lhsT=vp[:, kb, ho:ho + 64], rhs=wt[t % NW][:],
                                                      start=(kk == 0), stop=(kk == nkb - 1)),
                             reads=['vp', f'wt_{t % NW}'], writes=[f'OT{s}'])
                        if kk == nkb - 1:
                            h = pr * 2 + s
                            P.op('act', lambda e: e.activation(out=oT_all[:, h, qcols], in_=OT[s][0:64, :],
                                                               func=AF.Copy),
                                 reads=[f'OT{s}'], writes=['oT_all'])

                    for n in range(-2, NT + 2):
                        if 0 <= n + 2 < NT:
                            QK(n + 2)
                        if 0 <= n + 1 < NT:
                            EXP1(n + 1)
                        if 0 <= n < NT:
                            LN(n)
                            TRI(n)
                        if 0 <= n - 1 < NT:
                            EXPR(n - 1)
                            WMUL(n - 1)
                        if 0 <= n - 2 < NT:
                            AV(n - 2)
                        for _ in range(N_WARM):
                            P.op('pe', lambda e: e.matmul(DZ[:], lhsT=NTi, rhs=cst[:, 0:512], start=True, stop=True),
                                 reads=['cst'], writes=['DZ'])
            P.barrier()
            P.flush()

        s_q.close()
        if stop_after <= 3:
            with ExitStack() as ph:
                t32 = ph.enter_context(nc.sbuf_tensor("d3_dbt32", [64, 8, 2048], F32))
                P.op('dve', lambda e: e.tensor_copy(out=t32[:], in_=oT_all[:]), reads=['oT_all'], writes=['dbt32'])
                P.dma(dbg_d, t32[:], reads=['dbt32'], writes=['dbg'])
                P.wait_all_dma()
                P.barrier()
                P.flush()
            s_o.close()
            return nc

        with ExitStack() as ph:
            def T(name, shape, dt):
                return ph.enter_context(nc.sbuf_tensor("p2_" + name, list(shape), dt))

            def PS(name, shape, dt):
                return ph.enter_context(nc.psum_tensor("ps2_" + name, list(shape), dt))

            wp = WPool(ph, 3, 2, tag="w4")
            hres = T("hres", [128, 4, D], F32)
            xh = T("xh", [HALO, D], F32)
            junk = T("junk", [128, D], BF16)
            sst = [T(f"sst{i}", [128, 4], F32) for i in range(2)]
            xn = [T(f"xn{i}", [128, D], BF16) for i in range(2)]
            hnTs = T("hnTs", [128, 8, SLOT + HALO], BF16)
            glu = T("glu", [128, 4, SLOT + HALO], F32)
            acc = T("acc", [128, 4, SLOT], F32)
            hb = T("hb", [128, 4, SLOT], BF16)
            hsq = T("hsq", [128, 4, SLOT], BF16)
            ycT = T("ycT", [128, 4, SLOT], BF16)
            mix = T("mix", [128, 8, SLOT], F32)
            mixT = T("mixT", [128, 8, SLOT], BF16)
            tmp = [T(f"tmp{i}", [128, SLOT], F32) for i in range(4)]
            lnst = [T(f"lnst{i}", [128, SLOT], F32) for i in range(3)]
            ptr = [PS(f"ptr{i}", [128, 1024], BF16) for i in range(2)]
            psA = [PS(f"psA{i}", [128, 512], F32) for i in range(2)]
            psG = [PS(f"psG{i}", [128, 512], F32) for i in range(2)]
            psH = PS("psH", [128, 512], F32)
            psM = PS("psM", [128, 512], F32)

            for j in range(NSLOT):
                P.dma(xh[:], xq[j, 0:HALO, :], writes=['xh'])
                P.dma(hres[:], xq[j, HALO:HALO + SLOT, :].rearrange("(s p) n -> p s n", p=128), writes=['hres'])
                for sb in range(-1, 4):
                    b = (sb + 1) % 2
                    if sb < 0:
                        R, xin_ap, xres, col0 = HALO, xh[:], 'xh', 0
                    else:
                        R, xin_ap, xres, col0 = 128, hres[:, sb, :], 'hres', HALO + sb * 128
                    rms_rows(ph, xin_ap, R, xn[b][:R, :], sst[b], xres, f'xn{b}', junk, f'sst{b}')
                    for c in range(8):
                        P.op('pe', lambda e, c=c, R=R, b=b: e.transpose(out=ptr[b][:, c * 128:c * 128 + R],
                                                                        in_=xn[b][:R, c * 128:(c + 1) * 128],
                                                                        identity=cst[:R, 0:R]),
                             reads=[f'xn{b}', 'cst'], writes=[f'ptr{b}'])
                    P.op('act', lambda e, R=R, b=b, col0=col0: e.activation(
                        out=hnTs[:, :, col0:col0 + R],
                        in_=ptr[b][:].rearrange("p (c n) -> p c n", c=8)[:, :, 0:R], func=AF.Copy),
                         reads=[f'ptr{b}'], writes=['hnTs'])
                if p4_stop <= 1:
                    P.dma(h1_scr[j * 512:(j + 1) * 512, :].rearrange("(s p) n -> p s n", p=128), hres[:],
                          reads=['hres'], writes=['h1_scr'])
                    continue
                wa, wa_r = wp.load(w_in[:, 0:512], scale=g1t)
                wg_, wg_r = wp.load(w_in[:, 512:1024], scale=g1t)
                for cc in range(4):
                    pb_ = cc % 2
                    for (wt_, wr_, pst, hcol) in ((wa, wa_r, psA, 0), (wg_, wg_r, psG, 32)):
                        for c in range(8):
                            P.op('pe', lambda e, c=c, wt_=wt_, pst=pst: e.matmul(
                                pst[pb_][:], lhsT=wt_[:, c, cc * 128:(cc + 1) * 128], rhs=hnTs[:, c, HALO:HALO + SLOT],
                                start=(c == 0), stop=(c == 7)),
                                 reads=[wr_, 'hnTs'], writes=[f'{"psA" if pst is psA else "psG"}{pb_}'])
                        for c in range(8):
                            P.op('pe', lambda e, c=c, wt_=wt_, hcol=hcol: e.matmul(
                                psH[:, cc * 64 + hcol:cc * 64 + hcol + 32], lhsT=wt_[:, c, cc * 128:(cc + 1) * 128],
                                rhs=hnTs[:, c, 0:HALO], start=(c == 0), stop=(c == 7)),
                                 reads=[wr_, 'hnTs'], writes=['psH'])
                    tb = tmp[cc % 2]
                    P.op('act', lambda e, tb=tb: e.activation(out=tb[:], in_=psG[pb_][:], func=AF.Sigmoid),
                         reads=[f'psG{pb_}'], writes=[f'tmp{cc % 2}'])
                    P.op('dve', lambda e, tb=tb: e.tensor_tensor(out=glu[:, cc, HALO:HALO + SLOT], in0=psA[pb_][:],
                                                                 in1=tb[:], op=ALU.mult),
                         reads=[f'psA{pb_}', f'tmp{cc % 2}'], writes=[f'glu{cc}'])
                    tb2 = tmp[2 + cc % 2]
                    P.op('act', lambda e, tb2=tb2: e.activation(out=tb2[:, 0:32], in_=psH[:, cc * 64 + 32:cc * 64 + 64],
                                                                func=AF.Sigmoid),
                         reads=['psH'], writes=[f'tmp{2 + cc % 2}'])
                    P.op('dve', lambda e, tb2=tb2: e.tensor_tensor(out=glu[:, cc, 0:HALO], in0=psH[:, cc * 64:cc * 64 + 32],
                                                                   in1=tb2[:, 0:32], op=ALU.mult),
                         reads=['psH', f'tmp{2 + cc % 2}'], writes=[f'glu{cc}'])
                if p4_stop <= 2:
                    P.dma(h1_scr[j * 512:(j + 1) * 512, :].rearrange("(s p) n -> p s n", p=128), hres[:],
                          reads=['hres'], writes=['h1_scr'])
                    continue
                for cc in range(4):
                    eng = 'dve'
                    cw0 = C_CW + cc * 31
                    P.op(eng, lambda e, cw0=cw0: e.tensor_scalar(out=acc[:, cc, :], in0=glu[:, cc, 2:2 + SLOT],
                                                                 scalar1=spk[:, cw0:cw0 + 1],
                                                                 scalar2=spk[:, C_CB + cc:C_CB + cc + 1],
                                                                 op0=ALU.mult, op1=ALU.add),
                         reads=[f'glu{cc}', 'spk'], writes=[f'acc{cc}'])
                    for w in range(1, 31):
                        P.op(eng, lambda e, cw0=cw0, w=w: e.scalar_tensor_tensor(
                            out=acc[:, cc, :], in0=glu[:, cc, 2 + w:2 + w + SLOT], scalar=spk[:, cw0 + w:cw0 + w + 1],
                            in1=acc[:, cc, :], op0=ALU.mult, op1=ALU.add),
                             reads=[f'glu{cc}', 'spk', f'acc{cc}'], writes=[f'acc{cc}'])
                if p4_stop <= 3:
                    P.dma(h1_scr[j * 512:(j + 1) * 512, :].rearrange("(s p) n -> p s n", p=128), hres[:],
                          reads=['hres'], writes=['h1_scr'])
                    continue
                for cc in range(4):
                    P.op('act', lambda e: e.activation(out=hb[:, cc, :], in_=acc[:, cc, :], func=AF.Copy),
                         reads=[f'acc{cc}'], writes=[f'hb{cc}'])
                    P.op('act', lambda e: e.activation(out=hsq[:, cc, :], in_=acc[:, cc, :], func=AF.Square),
                         reads=[f'acc{cc}'], writes=[f'hsq{cc}'])
                for cc in range(4):
                    P.op('pe', lambda e: e.matmul(psM[:], lhsT=onesm, rhs=hb[:, cc, :], start=(cc == 0), stop=(cc == 3)),
                         reads=[f'hb{cc}', 'cst'], writes=['psM'])
                for cc in range(4):
                    P.op('pe', lambda e: e.matmul(psH[:], lhsT=onesm, rhs=hsq[:, cc, :], start=(cc == 0), stop=(cc == 3)),
                         reads=[f'hsq{cc}', 'cst'], writes=['psH'])
                P.op('act', lambda e: e.activation(out=lnst[0][:], in_=psM[:], func=AF.Copy),
                     reads=['psM'], writes=['lnst0'])
                P.op('act', lambda e: e.activation(out=lnst[1][:], in_=psM[:], func=AF.Square),
                     reads=['psM'], writes=['lnst1'])
                P.op('dve', lambda e: e.tensor_tensor(out=lnst[1][:], in0=psH[:], in1=lnst[1][:], op=ALU.subtract),
                     reads=['psH', 'lnst1'], writes=['lnst1'])
                P.op('act', lambda e: e.activation(out=lnst[2][:], in_=lnst[1][:], func=AF.Ln, bias=EPS, scale=1.0),
                     reads=['lnst1'], writes=['lnst2'])
                P.op('act', lambda e: e.activation(out=lnst[2][:], in_=lnst[2][:], func=AF.Exp, scale=-0.5),
                     reads=['lnst2'], writes=['lnst2'])
                for cc in range(4):
                    tb = tmp[cc % 2]
                    P.op('dve', lambda e, tb=tb: e.tensor_tensor(out=tb[:], in0=acc[:, cc, :], in1=lnst[0][:],
                                                                 op=ALU.subtract),
                         reads=[f'acc{cc}', 'lnst0'], writes=[f'tmp{cc % 2}'])
                    P.op('dve', lambda e, tb=tb: e.tensor_tensor(out=tb[:], in0=tb[:], in1=lnst[2][:], op=ALU.mult),
                         reads=[f'tmp{cc % 2}', 'lnst2'], writes=[f'tmp{cc % 2}'])
                    P.op('act', lambda e, tb=tb: e.activation(out=ycT[:, cc, :], in_=tb[:], func=AF.Silu,
                                                              bias=spk[:, C_LB + cc:C_LB + cc + 1],
                                                              scale=spk[:, C_LG + cc:C_LG + cc + 1]),
                         reads=[f'tmp{cc % 2}', 'spk'], writes=[f'ycT{cc}'])
                if p4_stop <= 4:
                    P.dma(h1_scr[j * 512:(j + 1) * 512, :].rearrange("(s p) n -> p s n", p=128), hres[:],
                          reads=['hres'], writes=['h1_scr'])
                    continue
                for part in range(2):
                    for half in range(2):
                        wgt, wgt_r = wp.load(w_gate[:, part * 1024 + half * 512:part * 1024 + (half + 1) * 512],
                                             scale=g1t)
                        if part == 0:
                            wy, wy_r = wp.load(w_conv_out[:, half * 512:(half + 1) * 512], kc=4)
                        else:
                            wy, wy_r = wp.load(w_sb_out[:, half * 512:(half + 1) * 512], kc=8, kp=64)
                        for mm in range(4):
                            m = half * 4 + mm
                            pb_ = m % 2
                            for c in range(8):
                                P.op('pe', lambda e, c=c: e.matmul(psG[pb_][:], lhsT=wgt[:, c, mm * 128:(mm + 1) * 128],
                                                                   rhs=hnTs[:, c, HALO:HALO + SLOT],
                                                                   start=(c == 0), stop=(c == 7)),
                                     reads=[wgt_r, 'hnTs'], writes=[f'psG{pb_}'])
                            if part == 0:
                                for cc in range(4):
                                    P.op('pe', lambda e, cc=cc: e.matmul(psA[pb_][:],
                                                                         lhsT=wy[:, cc, mm * 128:(mm + 1) * 128],
                                                                         rhs=ycT[:, cc, :], start=(cc == 0), stop=(cc == 3)),
                                         reads=[wy_r, f'ycT{cc}'], writes=[f'psA{pb_}'])
                            else:
                                for h in range(8):
                                    P.op('pe', lambda e, h=h: e.matmul(psA[pb_][:],
                                                                       lhsT=wy[0:64, h, mm * 128:(mm + 1) * 128],
                                                                       rhs=oT_all[:, h, j * 512:(j + 1) * 512],
                                                                       start=(h == 0), stop=(h == 7)),
                                         reads=[wy_r, 'oT_all'], writes=[f'psA{pb_}'])
                            tb = tmp[m % 2]
                            bcol = C_BG + part * 8 + m
                            P.op('act', lambda e, tb=tb, bcol=bcol: e.activation(out=tb[:], in_=psG[pb_][:],
                                                                                 func=AF.Sigmoid,
                                                                                 bias=spk[:, bcol:bcol + 1], scale=1.0),
                                 reads=[f'psG{pb_}', 'spk'], writes=[f'tmp{m % 2}'])
                            if part == 0:
                                P.op('dve', lambda e, tb=tb: e.tensor_tensor(out=mix[:, m, :], in0=psA[pb_][:], in1=tb[:],
                                                                             op=ALU.mult),
                                     reads=[f'psA{pb_}', f'tmp{m % 2}'], writes=[f'mix{m}'])
                            else:
                                P.op('dve', lambda e, tb=tb: e.tensor_tensor(out=tb[:], in0=psA[pb_][:], in1=tb[:],
                                                                             op=ALU.mult),
                                     reads=[f'psA{pb_}', f'tmp{m % 2}'], writes=[f'tmp{m % 2}'])
                                P.op('pool', lambda e, tb=tb: e.tensor_tensor(out=mixT[:, m, :], in0=mix[:, m, :],
                                                                              in1=tb[:], op=ALU.add),
                                     reads=[f'mix{m}', f'tmp{m % 2}'], writes=[f'mixT{m}'])
                if p4_stop <= 5:
                    P.dma(h1_scr[j * 512:(j + 1) * 512, :].rearrange("(s p) n -> p s n", p=128), hres[:],
                          reads=['hres'], writes=['h1_scr'])
                    continue
                for half in range(2):
                    wo, wo_r = wp.load(w_o[:, half * 512:(half + 1) * 512])
                    for sb in range(4):
                        pb_ = sb % 2
                        for m in range(8):
                            P.op('pe', lambda e, m=m: e.matmul(psA[pb_][:], lhsT=mixT[:, m, sb * 128:(sb + 1) * 128],
                                                               rhs=wo[:, m, :], start=(m == 0), stop=(m == 7)),
                                 reads=[wo_r, f'mixT{m}'], writes=[f'psA{pb_}'])
                        P.op('dve', lambda e: e.tensor_tensor(out=hres[:, sb, half * 512:(half + 1) * 512],
                                                              in0=psA[pb_][:],
                                                              in1=hres[:, sb, half * 512:(half + 1) * 512], op=ALU.add),
                             reads=[f'psA{pb_}', 'hres'], writes=['hres'])
                P.dma(h1_scr[j * 512:(j + 1) * 512, :].rearrange("(s p) n -> p s n", p=128), hres[:],
                      reads=['hres'], writes=['h1_scr'])
            P.wait_all_dma()
            P.barrier()
            P.flush()

        s_o.close()
        if stop_after <= 4:
            with ExitStack() as ph:
                t32 = ph.enter_context(nc.sbuf_tensor("d4_dbt32", [128, NBLK, D], F32))
                P.dma(t32[:], h1_scr.rearrange("(s p) n -> p s n", p=128), reads=['h1_scr'], writes=['dbt32'])
                P.dma(out_d.rearrange("(s p) n -> p s n", p=128), t32[:], reads=['dbt32'], writes=['out'])
                P.wait_all_dma()
                P.barrier()
                P.flush()
            return nc

        hn2T = GT("hn2T", [128, 8, NBLK * 128], BF16)
        with ExitStack() as ph:
            def T(name, shape, dt):
                return ph.enter_context(nc.sbuf_tensor("p3_" + name, list(shape), dt))

            def PS(name, shape, dt):
                return ph.enter_context(nc.psum_tensor("ps3_" + name, list(shape), dt))

            wp = WPool(ph, 2, 1, tag="w5")
            kkT = T("kkT", [128, 16, 128], BF16)
            qpT = T("qpT", [128, 16, SLOT], BF16)
            S = T("S", [128, 16, 128], F32)
            wk1 = T("wk1", [128, 256], F32)
            V16 = T("V16", [128, 16, 16], F32)
            cand = T("cand", [128, 2, 256], F32)
            best = T("best", [128, 8, 16], F32)
            st5 = T("st5", [128, 64], F32)
            junk16 = T("junk16", [128, 16], F32)
            E12 = T("E12", [128, 16, 128], F32)
            psq = [PS(f"psq{i}", [128, 512], F32) for i in range(1)] * 2
            pss = [PS(f"pss{i}", [128, 512], F32) for i in range(1)] * 2

            s_k = ExitStack()
            kkf = s_k.enter_context(nc.sbuf_tensor("p5_kkf", [128, 16, 128], F32))
            kkb = s_k.enter_context(nc.sbuf_tensor("p5_kkb", [128, 16, 128], BF16))
            xin = [s_k.enter_context(nc.sbuf_tensor(f"p5_xin{i}", [128, D], F32)) for i in range(2)]
            junk = s_k.enter_context(nc.sbuf_tensor("p5_junk", [128, D], BF16))
            sst = [s_k.enter_context(nc.sbuf_tensor(f"p5_sst{i}", [128, 4], F32)) for i in range(2)]
            xn = [s_k.enter_context(nc.sbuf_tensor(f"p5_xn{i}", [128, D], BF16)) for i in range(2)]
            ptr = [s_k.enter_context(nc.psum_tensor(f"ps5_ptr{i}", [128, 1024], BF16)) for i in range(2)]
            P.dma(kkf[:], peer_k.rearrange("q k d -> k q d"), writes=['kkf'])
            P.op('dve', lambda e: e.tensor_copy(out=kkb[:], in_=kkf[:]), reads=['kkf'], writes=['kkb'])
            for qi in range(16):
                b = (qi // 8) % 2
                P.op('pe', lambda e, qi=qi, b=b: e.transpose(out=ptr[b][:, (qi % 8) * 128:(qi % 8 + 1) * 128],
                                                             in_=kkb[:, qi, :], identity=ident),
                     reads=['kkb', 'cst'], writes=[f'ptr{b}'])
                if qi % 8 == 7:
                    P.op('act', lambda e, qi=qi, b=b: e.activation(
                        out=kkT[:, qi - 7:qi + 1, :].rearrange("p a n -> p (a n)"), in_=ptr[b][:], func=AF.Copy),
                         reads=[f'ptr{b}'], writes=['kkT'])
            for blk in range(NBLK):
                b = blk % 2
                P.dma(xin[b][:], h1_scr[blk * 128:(blk + 1) * 128, :], reads=['h1_scr'], writes=[f'xin{b}'])
                rms_rows(ph, xin[b][:], 128, xn[b][:], sst[b], f'xin{b}', f'xn{b}', junk, f'sst{b}')
                for c in range(8):
                    P.op('pe', lambda e, c=c, b=b: e.transpose(out=ptr[b][:, c * 128:(c + 1) * 128],
                                                               in_=xn[b][:, c * 128:(c + 1) * 128], identity=ident),
                         reads=[f'xn{b}', 'cst'], writes=[f'ptr{b}'])
                P.op('act', lambda e, b=b, blk=blk: e.activation(
                    out=hn2T[:, :, blk * 128:(blk + 1) * 128],
                    in_=ptr[b][:].rearrange("p (c n) -> p c n", c=8), func=AF.Copy),
                     reads=[f'ptr{b}'], writes=['hn2T'])
            P.barrier()
            P.flush()
            s_k.close()
            ebuf = [T(f"ebuf{i}", [128, 2048], F32) for i in range(2)]
            ebufA = [[T(f"ebufA{i}_{a}", [128, 2048], F32) for a in range(len(ACT_HEADS))] for i in range(2)]
            csum = T("csum", [128, 2048], BF16)
            NCM = 6
            cm = [T(f"cm{i}", [128, 2048], BF16) for i in range(NCM)]
            ctb = [T(f"ctb{i}", [128, 16, 128], BF16) for i in range(2)]
            pacc = [PS(f"pacc{i}", [128, 512], F32) for i in range(4)]
            pct = PS("pct", [128, 2048], BF16)

            for tg in range(4):
                for piece in range(4):
                    wqp, wqp_r = wp.load(peer_wq[:, piece * 512:(piece + 1) * 512], scale=g2t)
                    for qq in range(4):
                        qi = piece * 4 + qq
                        b = qi % 2
                        for c in range(8):
                            P.op('pe', lambda e, c=c, qq=qq, b=b: e.matmul(
                                psq[b][:], lhsT=wqp[:, c, qq * 128:(qq + 1) * 128],
                                rhs=hn2T[:, c, tg * 512:(tg + 1) * 512], start=(c == 0), stop=(c == 7)),
                                 reads=[wqp_r, 'hn2T'], writes=['psq0'])
                        P.op('act', lambda e, qi=qi, b=b: e.activation(out=qpT[:, qi, :], in_=psq[b][:], func=AF.Copy),
                             reads=['psq0'], writes=[f'qpT{qi}'])
                for bb in range(4):
                    blk = tg * 4 + bb
                    for g4 in range(4):
                        b = g4 % 2
                        for qq in range(4):
                            qi = g4 * 4 + qq
                            P.op('pe', lambda e, qi=qi, qq=qq, b=b: e.matmul(
                                pss[b][:, qq * 128:(qq + 1) * 128], lhsT=qpT[:, qi, bb * 128:(bb + 1) * 128],
                                rhs=kkT[:, qi, :], start=True, stop=True),
                                 reads=[f'qpT{qi}', 'kkT'], writes=['pss0'])
                        P.op('act', lambda e, g4=g4, b=b: e.activation(
                            out=S[:, g4 * 4:(g4 + 1) * 4, :].rearrange("p a n -> p (a n)"), in_=pss[b][:], func=AF.Copy),
                             reads=['pss0'], writes=['S'])
                    for qi in range(16):
                        P.op('dve', lambda e, qi=qi: e.max(out=V16[:, qi, 0:8], in_=S[:, qi, :]),
                             reads=['S'], writes=['V16'])
                        P.op('dve', lambda e, qi=qi: e.match_replace(out=wk1[:, 0:128], in_to_replace=V16[:, qi, 0:8],
                                                                     in_values=S[:, qi, :], imm_value=NEG),
                             reads=['S', 'V16'], writes=['wk1'])
                        P.op('dve', lambda e, qi=qi: e.max(out=V16[:, qi, 8:16], in_=wk1[:, 0:128]),
                             reads=['wk1'], writes=['V16'])
                    for h in range(8):
                        P.op('dve', lambda e, h=h: e.tensor_tensor(
                            out=cand[:, h % 2, :].rearrange("p (a b) -> p a b", a=16),
                            in0=V16[:, 2 * h, :].unsqueeze(2).to_broadcast([128, 16, 16]),
                            in1=V16[:, 2 * h + 1, :].unsqueeze(1).to_broadcast([128, 16, 16]), op=ALU.add),
                             reads=['V16'], writes=[f'cand{h % 2}'])
                        P.op('dve', lambda e, h=h: e.max(out=best[:, h, 0:8], in_=cand[:, h % 2, :]),
                             reads=[f'cand{h % 2}'], writes=['best'])
                        P.op('dve', lambda e, h=h: e.match_replace(out=wk1[:], in_to_replace=best[:, h, 0:8],
                                                                   in_values=cand[:, h % 2, :], imm_value=NEG),
                             reads=[f'cand{h % 2}', 'best'], writes=['wk1'])
                        P.op('dve', lambda e, h=h: e.max(out=best[:, h, 8:16], in_=wk1[:]),
                             reads=['wk1'], writes=['best'])
                    V4 = V16[:].rearrange("p (h t) k -> p h t k", t=2)
                    P.op('dve', lambda e: e.tensor_scalar(out=st5[:, 0:8], in0=best[:, :, 0], scalar1=-1.0, scalar2=None,
                                                          op0=ALU.mult),
                         reads=['best'], writes=['st5a'])
                    for h in range(8):
                        P.op('act', lambda e, h=h: e.activation(out=junk16[:], in_=best[:, h, :], func=AF.Exp,
                                                                bias=st5[:, h:h + 1], scale=1.0,
                                                                accum_out=st5[:, 8 + h:9 + h]),
                             reads=['best', 'st5a'], writes=['junk16', 'st5b'])
                    P.op('act', lambda e: e.activation(out=st5[:, 16:24], in_=st5[:, 8:16], func=AF.Ln),
                         reads=['st5b'], writes=['st5c'])
                    P.op('dve', lambda e: e.tensor_scalar(out=st5[:, 24:32], in0=V4[:, :, 0, 0], scalar1=-1.0,
                                                          scalar2=None, op0=ALU.mult),
                         reads=['V16'], writes=['st5d'])
                    P.op('dve', lambda e: e.scalar_tensor_tensor(out=st5[:, 32:40], in0=V4[:, :, 1, 0], scalar=-1.0,
                                                                 in1=st5[:, 16:24], op0=ALU.mult, op1=ALU.subtract),
                         reads=['V16', 'st5c'], writes=['st5e'])
                    P.op('dve', lambda e: e.tensor_tensor(out=st5[:, 40:48], in0=best[:, :, 15], in1=st5[:, 0:8],
                                                          op=ALU.add),
                         reads=['best', 'st5a'], writes=['st5f'])
                    P.op('dve', lambda e: e.tensor_tensor(out=st5[:, 40:48], in0=st5[:, 40:48], in1=st5[:, 16:24],
                                                          op=ALU.subtract),
                         reads=['st5f', 'st5c'], writes=['st5f'])
                    P.op('act', lambda e: e.activation(out=st5[:, 48:56], in_=st5[:, 40:48], func=AF.Exp),
                         reads=['st5f'], writes=['st5g'])
                    P.op('dve', lambda e: e.tensor_scalar(out=st5[:, 48:56], in0=st5[:, 48:56], scalar1=1.0 - 1e-4,
                                                          scalar2=None, op0=ALU.mult),
                         reads=['st5g'], writes=['st5g'])
                    for h in range(8):
                        P.op('act', lambda e, h=h: e.activation(out=E12[:, 2 * h, :], in_=S[:, 2 * h, :], func=AF.Exp,
                                                                bias=st5[:, 24 + h:25 + h], scale=1.0),
                             reads=['S', 'st5d'], writes=['E12'])
                        P.op('act', lambda e, h=h: e.activation(out=E12[:, 2 * h + 1, :], in_=S[:, 2 * h + 1, :],
                                                                func=AF.Exp, bias=st5[:, 32 + h:33 + h], scale=1.0),
                             reads=['S', 'st5e'], writes=['E12'])
                    def act_outer(q8_):
                        for ai, h in enumerate(ACT_HEADS):
                            eb = ebufA[q8_ % 2][ai]
                            for il in range(16):
                                i1c = q8_ * 16 + il
                                P.op('act', lambda e, h=h, il=il, i1c=i1c, eb=eb: e.activation(
                                    out=eb[:, il * 128:(il + 1) * 128], in_=E12[:, 2 * h + 1, :], func=AF.Copy,
                                    scale=E12[:, 2 * h, i1c:i1c + 1]),
                                     reads=['E12'], writes=[f'ebufA{q8_ % 2}_{ai}'])

                    act_outer(0)
                    for q8 in range(8):
                        cs_ = q8 % 2
                        if q8 + 1 < 8:
                            act_outer(q8 + 1)
                        for hi, h in enumerate(HEAD_ORDER):
                            kc_ = (q8 * 8 + hi) % NCM
                            if h in ACT_HEADS:
                                ai = ACT_HEADS.index(h)
                                src, sres = ebufA[cs_][ai], f'ebufA{cs_}_{ai}'
                            else:
                                k_ = (q8 * 8 + hi) % 2
                                src, sres = ebuf[k_], f'ebuf{k_}'
                                P.op('dve', lambda e, h=h, src=src: e.tensor_tensor(
                                    out=src[:].rearrange("p (a b) -> p a b", a=16),
                                    in0=E12[:, 2 * h, q8 * 16:(q8 + 1) * 16].unsqueeze(2).to_broadcast([128, 16, 128]),
                                    in1=E12[:, 2 * h + 1, :].unsqueeze(1).to_broadcast([128, 16, 128]), op=ALU.mult),
                                     reads=['E12'], writes=[sres])
                            P.op('dve', lambda e, h=h, src=src, kc_=kc_: e.scalar_tensor_tensor(
                                out=cm[kc_][:], in0=src[:], scalar=st5[:, 48 + h:49 + h], in1=src[:],
                                op0=ALU.is_ge, op1=ALU.mult),
                                 reads=[sres, 'st5g'], writes=[f'cm{kc_}'])
                            for jb in range(4):
                                P.op('pe', lambda e, jb=jb, kc_=kc_, hi=hi: e.matmul(
                                    pacc[jb][:], lhsT=ident, rhs=cm[kc_][:, jb * 512:(jb + 1) * 512],
                                    start=(hi == 0), stop=(hi == 7)),
                                     reads=[f'cm{kc_}', 'cst'], writes=[f'pacc{jb}'])
                        for jb in range(4):
                            P.op('act', lambda e, jb=jb: e.activation(out=csum[:, jb * 512:(jb + 1) * 512], in_=pacc[jb][:],
                                                                      func=AF.Copy),
                                 reads=[f'pacc{jb}'], writes=[f'csum{jb}'])
                        for c in range(16):
                            P.op('pe', lambda e, c=c: e.transpose(out=pct[:, c * 128:(c + 1) * 128],
                                                                  in_=csum[:, c * 128:(c + 1) * 128], identity=ident),
                                 reads=[f'csum{c // 4}', 'cst'], writes=[f'pct{c // 8}'])
                        for b2 in range(2):
                            P.op('act', lambda e, b2=b2: e.activation(
                                out=ctb[cs_][:, b2 * 8:(b2 + 1) * 8, :].rearrange("p a n -> p (a n)"),
                                in_=pct[:, b2 * 1024:(b2 + 1) * 1024], func=AF.Copy),
                                 reads=[f'pct{b2}'], writes=[f'ctb{cs_}'])
                        P.dma(ct_scr[q8 * 16:(q8 + 1) * 16, :, blk * 128:(blk + 1) * 128].rearrange("a p n -> p a n"),
                              ctb[cs_][:], reads=[f'ctb{cs_}'], writes=['ct_scr'])
            P.wait_all_dma()
            P.barrier()
            P.flush()

        with ExitStack() as ph:
            def T(name, shape, dt):
                return ph.enter_context(nc.sbuf_tensor("p4_" + name, list(shape), dt))

            def PS(name, shape, dt):
                return ph.enter_context(nc.psum_tensor("ps4_" + name, list(shape), dt))

            G = 2
            oacc = T("oacc", [128, NBLK, D], F32)
            uf = [T(f"uf{i}", [128, G, D], F32) for i in range(2)]
            vf = [T(f"vf{i}", [128, G, D], F32) for i in range(2)]
            ub = T("ub", [128, G, D], BF16)
            vbb = [T(f"vbb{i}", [128, G, D], BF16) for i in range(4)]
            uT = [T(f"uT{i}", [128, G, 8, 128], BF16) for i in range(2)]
            ctg = [T(f"ctg{i}", [128, G, NBLK * 128], BF16) for i in range(2)]
            coefT = T("coefT", [128, 2 * G, NBLK * 128], BF16)
            gl = [T(f"gl{i}", [128, 512], F32) for i in range(2)]
            ptr = [PS(f"ptr{i}", [128, 1024], BF16) for i in range(2)]
            psa = [PS(f"psa{i}", [128, 512], F32) for i in range(2)]
            pso = [PS(f"pso{i}", [128, 512], F32) for i in range(2)]

            P.dma(oacc[:], h1_scr.rearrange("(s p) n -> p s n", p=128), reads=['h1_scr'], writes=['oacc'])
            NG = 128 // G
            for g in range(NG):
                b = g % 2
                vb4 = g % 4
                sg = g % 2
                e0 = g * G * 128
                P.dma(uf[b][:], peer_u[e0:e0 + G * 128, :].rearrange("(a p) n -> p a n", p=128), writes=[f'uf{b}'])
                P.dma(vf[b][:], peer_v[e0:e0 + G * 128, :].rearrange("(a p) n -> p a n", p=128), writes=[f'vf{b}'])
                P.dma(ctg[b][:], ct_scr[g * G:(g + 1) * G, :, :].rearrange("a p n -> p a n"), reads=['ct_scr'],
                      writes=[f'ctg{b}'])
                P.op('pool', lambda e, b=b: e.tensor_copy(out=ub[:], in_=uf[b][:]), reads=[f'uf{b}'], writes=['ub'])
                P.op('pool', lambda e, b=b, vb4=vb4: e.tensor_copy(out=vbb[vb4][:], in_=vf[b][:]), reads=[f'vf{b}'],
                     writes=[f'vbb{vb4}'])
                for a in range(G):
                    pb_ = a % 2
                    for c in range(8):
                        P.op('pe', lambda e, a=a, c=c, pb_=pb_: e.transpose(out=ptr[pb_][:, c * 128:(c + 1) * 128],
                                                                            in_=ub[:, a, c * 128:(c + 1) * 128],
                                                                            identity=ident),
                             reads=['ub', 'cst'], writes=[f'ptr{pb_}'])
                    P.op('dve', lambda e, a=a, b=b, pb_=pb_: e.tensor_tensor(
                        out=uT[b][:, a, :, :], in0=ptr[pb_][:].rearrange("p (c n) -> p c n", c=8),
                        in1=g2t.unsqueeze(2).to_broadcast([128, 8, 128]), op=ALU.mult),
                         reads=[f'ptr{pb_}', 'spk'], writes=[f'uT{b}_{a}'])
                for a in range(G):
                    for tg in range(4):
                        k = (a * 4 + tg) % 2
                        for c in range(8):
                            P.op('pe', lambda e, a=a, c=c, tg=tg, k=k, b=b: e.matmul(
                                psa[k][:], lhsT=uT[b][:, a, c, :], rhs=hn2T[:, c, tg * 512:(tg + 1) * 512],
                                start=(c == 0), stop=(c == 7)),
                                 reads=[f'uT{b}_{a}', 'hn2T'], writes=[f'psa{k}'])
                        P.op('act', lambda e, k=k: e.activation(out=gl[k][:], in_=psa[k][:], func=AF.Gelu),
                             reads=[f'psa{k}'], writes=[f'gl{k}'])
                        P.op('dve', lambda e, a=a, tg=tg, k=k, b=b: e.tensor_tensor(
                            out=coefT[:, sg * G + a, tg * 512:(tg + 1) * 512], in0=gl[k][:],
                            in1=ctg[b][:, a, tg * 512:(tg + 1) * 512], op=ALU.mult),
                             reads=[f'gl{k}', f'ctg{b}'], writes=[f'coefT{tg}'])
                if sg == 0:
                    continue
                for blk in range(NBLK):
                    for half in range(2):
                        k = (blk * 2 + half) % 2
                        for a4 in range(2 * G):
                            vsel = (g - 1 + a4 // G) % 4
                            P.op('pe', lambda e, a4=a4, blk=blk, half=half, k=k, vsel=vsel: e.matmul(
                                pso[k][:], lhsT=coefT[:, a4, blk * 128:(blk + 1) * 128],
                                rhs=vbb[vsel][:, a4 % G, half * 512:(half + 1) * 512],
                                start=(a4 == 0), stop=(a4 == 2 * G - 1)),
                                 reads=[f'coefT{blk // 4}', f'vbb{vsel}'], writes=[f'pso{k}'])
                        P.op('dve', lambda e, blk=blk, half=half, k=k: e.tensor_tensor(
                            out=oacc[:, blk, half * 512:(half + 1) * 512], in0=pso[k][:],
                            in1=oacc[:, blk, half * 512:(half + 1) * 512], op=ALU.add),
                             reads=[f'pso{k}', f'oacc{blk}_{half}', 'oacc'], writes=[f'oacc{blk}_{half}'])
            all_o = [f'oacc{blk}_{half}' for blk in range(NBLK) for half in range(2)]
            P.dma(out_d.rearrange("(s p) n -> p s n", p=128), oacc[:], reads=all_o + ['oacc'], writes=['out'])
            P.wait_all_dma()
            P.barrier()
            P.flush()
    return nc


def _host_prep(inputs):
    f32 = np.float32
    x = np.asarray(inputs["x"], f32)[0]
    meta = np.asarray(inputs["meta_tokens"], f32)
    hall = np.zeros((130 * 128, D), f32)
    hall[16:32] = meta
    hall[32:32 + SEQ] = x
    p = np.arange(128)
    cst = np.zeros((128, 640), f32)
    cst[:, 0:128] = np.eye(128)
    cst[:, 128:256] = -1.0 * (p[:, None] >= p[None, :])
    cst[:, 256:384] = -1.0 * (p[:, None] < p[None, :])
    cst[:, 384:512] = 1.0 / 512
    cst[:, 512:640] = -1.0
    cst = cst.astype(ml_dtypes.bfloat16)
    spk = np.zeros((128, C_END), f32)
    spk[:, C_G1:C_G1 + 8] = np.asarray(inputs["norm1_g"], f32)[0].reshape(8, 128).T
    spk[:, C_G2:C_G2 + 8] = np.asarray(inputs["norm2_g"], f32)[0].reshape(8, 128).T
    cw = np.asarray(inputs["conv_w"], f32)[0]
    spk[:, C_CW:C_CW + 124] = cw.reshape(31, 4, 128).transpose(2, 1, 0).reshape(128, 124)
    spk[:, C_CB:C_CB + 4] = np.asarray(inputs["conv_b"], f32)[0].reshape(4, 128).T
    spk[:, C_LG:C_LG + 4] = np.asarray(inputs["conv_ln_g"], f32)[0].reshape(4, 128).T
    spk[:, C_LB:C_LB + 4] = np.asarray(inputs["conv_ln_b"], f32)[0].reshape(4, 128).T
    spk[:, C_BG:C_BG + 16] = np.asarray(inputs["b_gate"], f32)[0].reshape(16, 128).T
    spk[:, C_GQ] = np.tile(np.asarray(inputs["q_norm_g"], f32)[0], 2)
    spk[:, C_GK] = np.tile(np.asarray(inputs["k_norm_g"], f32)[0], 2)
    peer_k = np.stack([np.asarray(inputs["peer_k1"], f32)[0], np.asarray(inputs["peer_k2"], f32)[0]], axis=1)
    peer_k = np.ascontiguousarray(peer_k.reshape(16, 128, 128))
    shared = {
        "hall": hall, "cst": cst, "spk": spk,
        "w_in": np.ascontiguousarray(np.asarray(inputs["w_in"], f32)[0]),
        "w_conv_out": np.ascontiguousarray(np.asarray(inputs["w_conv_out"], f32)[0]),
        "w_sb_out": np.ascontiguousarray(np.asarray(inputs["w_sb_out"], f32)[0]),
        "w_gate": np.ascontiguousarray(np.asarray(inputs["w_gate"], f32)[0]),
        "w_o": np.ascontiguousarray(np.asarray(inputs["w_o"], f32)[0]),
        "peer_wq": np.ascontiguousarray(np.asarray(inputs["peer_wq"], f32)[0]),
        "peer_k": peer_k,
        "peer_u": np.ascontiguousarray(np.asarray(inputs["peer_u"], f32)[0]),
        "peer_v": np.ascontiguousarray(np.asarray(inputs["peer_v"], f32)[0]),
    }
    in_maps = []
    kp = np.arange(128)[:, None]
    qn = np.arange(512)[None, :]
    for c in range(NCORE):
        m = dict(shared)
        xq = np.stack([hall[512 * (8 * j + c):512 * (8 * j + c) + SLOT + HALO] for j in range(NSLOT)])
        m["xq"] = np.ascontiguousarray(xq)
        am = np.stack([(128 * r + kp < 512 * c + 16 + qn) for r in range(33)], axis=1)
        m["amask"] = np.ascontiguousarray(am.reshape(128, 33 * 512).astype(f32).astype(ml_dtypes.bfloat16))
        in_maps.append(m)
    return in_maps


def _assemble(res):
    out = np.zeros((1, SEQ, D), np.float32)
    for c in range(NCORE):
        o = np.asarray(res.results[c]["out"], np.float32)
        for j in range(NSLOT):
            g = 8 * j + c
            out[0, 512 * g:512 * (g + 1)] = o[512 * j:512 * (j + 1)]
    return out


_NC_CACHE = {}


def kernel(**inputs):
    in_maps = _host_prep(inputs)
    if "nc" not in _NC_CACHE:
        _NC_CACHE["nc"] = build_program()
    res = run_bass_kernel_spmd(_NC_CACHE["nc"], in_maps, core_ids=list(range(NCORE)))
    return _assemble(res)
```

```python
import numpy as np
import ml_dtypes
from contextlib import ExitStack

import concourse.bass as bass
import concourse.mybir as mybir
from concourse.bass_utils import run_bass_kernel_spmd

F32 = mybir.dt.float32
BF16 = mybir.dt.bfloat16
AF = mybir.ActivationFunctionType
ALU = mybir.AluOpType
AX = mybir.AxisListType

NCORE = 8
D = 1024
SEQ = 16384
NMETA = 16
NKB = 129
LKP = NKB * 128
NSLOT = 4
SLOT = 512
HALO = 32
NBLK = 16
EPS = 1e-6
CH = 8192
ACT_HEADS = (1, 4, 7)
HEAD_ORDER = (0, 2, 1, 3, 5, 4, 6, 7)
N_WARM1 = 0
N_WARM = 0
NEG = -1.0e30

C_G1, C_G2, C_CW, C_CB, C_LG, C_LB, C_BG, C_GQ, C_GK, C_END = 0, 8, 16, 140, 144, 148, 152, 168, 169, 170


class _Rec:
    def __init__(self):
        self.call = None

    def __getattr__(self, name):
        def f(*a, **k):
            self.call = (name, a, k)
            return self
        return f


class Prog:
    ENG = ['pe', 'act', 'dve', 'pool', 'sp']

    def __init__(self, nc, stack, n_dma_sems=12):
        self.nc = nc
        self.stack = stack
        self.q = {e: [] for e in self.ENG}
        self.seq = {e: 0 for e in self.ENG}
        self.sems = {e: [] for e in self.ENG}
        self.waited = {e: {} for e in self.ENG}
        self.lastw = {}
        self.readers = {}
        self.ndma = n_dma_sems
        self.dma_sems = [stack.enter_context(nc.semaphore(f"dma{i}")) for i in range(n_dma_sems)]
        self.dma_count = 0
        self.dma_waited_set = {e: set() for e in self.ENG}
        self.nwait = 0

    def _sem(self, e, seq):
        i = (seq - 1) // CH
        while len(self.sems[e]) <= i:
            self.sems[e].append(self.stack.enter_context(self.nc.semaphore(f"s_{e}_{len(self.sems[e])}")))
        return self.sems[e][i], ((seq - 1) % CH) + 1

    def _need(self, e, dep):
        if dep[0] == 'dma':
            idx = dep[1]
            if idx in self.dma_waited_set[e]:
                return
            self.dma_waited_set[e].add(idx)
            sem = self.dma_sems[idx % self.ndma]
            val = 16 * (idx // self.ndma + 1)
            self.q[e].append(('wait', sem, val))
            self.nwait += 1
        else:
            _, pe_, seq = dep
            if self.waited[e].get(pe_, 0) >= seq:
                return
            self.waited[e][pe_] = seq
            sem, val = self._sem(pe_, seq)
            self.q[e].append(('wait', sem, val))
            self.nwait += 1

    def _deps(self, e, reads, writes):
        deps = []
        for r in reads:
            if r in self.lastw:
                deps.append(self.lastw[r])
        for w in writes:
            if w in self.lastw:
                d = self.lastw[w]
                if not (e == 'pe' and d[0] == 'eng' and d[1] == 'pe'):
                    deps.append(d)
            for d in self.readers.get(w, {}).values():
                if d[0] == 'eng' and d[1] == e and e == 'pe':
                    continue
                deps.append(d)
        return deps

    def op(self, e, fn, reads=(), writes=()):
        for d in self._deps(e, reads, writes):
            self._need(e, d)
        self.seq[e] += 1
        s = self.seq[e]
        sem, _ = self._sem(e, s)
        rec = _Rec()
        fn(rec)
        assert rec.call is not None
        self.q[e].append(('op', rec.call, sem))
        me = ('eng', e, s)
        for w in writes:
            self.lastw[w] = me
            self.readers[w] = {}
        for r in reads:
            if r not in writes:
                self.readers.setdefault(r, {})[e] = me
        return me

    def dma(self, out, in_, reads=(), writes=(), **kw):
        e = 'sp'
        idx = self.dma_count
        self.dma_count += 1
        if idx - self.ndma >= 0:
            self._need(e, ('dma', idx - self.ndma))
        for d in self._deps(e, reads, writes):
            self._need(e, d)
        sem = self.dma_sems[idx % self.ndma]
        self.q[e].append(('dma', out, in_, sem, kw))
        me = ('dma', idx)
        for w in writes:
            self.lastw[w] = me
            self.readers[w] = {}
        for r in reads:
            self.readers.setdefault(r, {})[('dma', idx)] = me
        return me

    def barrier(self):
        for e in self.ENG:
            for p in self.ENG:
                if p != e and self.seq[p] > 0:
                    self._need(e, ('eng', p, self.seq[p]))
            for idx in range(max(0, self.dma_count - self.ndma), self.dma_count):
                self._need(e, ('dma', idx))
        self.lastw = {}
        self.readers = {}

    def wait_all_dma(self, e='sp'):
        for idx in range(max(0, self.dma_count - self.ndma), self.dma_count):
            self._need(e, ('dma', idx))

    def flush(self):
        nc = self.nc
        q = self.q
        self.q = {e: [] for e in self.ENG}

        def run(eng, items):
            for it in items:
                if it[0] == 'wait':
                    eng.wait_ge(it[1], it[2])
                elif it[0] == 'op':
                    name, a, k = it[1]
                    getattr(eng, name)(*a, **k).then_inc(it[2], 1)
                else:
                    eng.dma_start(out=it[1], in_=it[2], **it[4]).then_inc(it[3], 16)

        with nc.Block() as block:
            @block.tensor
            def _(eng):
                run(eng, q['pe'])

            @block.scalar
            def _(eng):
                run(eng, q['act'])

            @block.vector
            def _(eng):
                run(eng, q['dve'])

            @block.gpsimd
            def _(eng):
                run(eng, q['pool'])

            @block.sync
            def _(eng):
                run(eng, q['sp'])


def build_program(stop_after=99, att_pairs=4, att_slots=4, dbg=None, p4_stop=99, skip_kv=False):
    nc = bass.Bass("TRN2", target_bir_lowering=False)

    def din(name, shape, dt=F32):
        return nc.dram_tensor(name, list(shape), dt, kind="ExternalInput").ap()

    hall = din("hall", [130 * 128, D])
    xq = din("xq", [NSLOT, SLOT + HALO, D])
    amask_d = din("amask", [128, 33 * 512], BF16)
    cst_d = din("cst", [128, 5 * 128], BF16)
    spk_d = din("spk", [128, C_END])
    w_in = din("w_in", [D, 2560])
    w_conv_out = din("w_conv_out", [512, D])
    w_sb_out = din("w_sb_out", [512, D])
    w_gate = din("w_gate", [D, 2048])
    w_o = din("w_o", [D, D])
    peer_wq = din("peer_wq", [D, 2048])
    peer_k = din("peer_k", [16, 128, 128])
    NE_ = 16384 if stop_after > 4 else 128
    peer_u = din("peer_u", [NE_, D])
    peer_v = din("peer_v", [NE_, D])
    out_d = nc.dram_tensor("out", [NBLK * 128, D], F32, kind="ExternalOutput").ap()
    dbg_d = None
    if dbg is not None:
        dbg_d = nc.dram_tensor("dbg", list(dbg), F32, kind="ExternalOutput").ap()

    kT_scr = nc.dram_tensor("kT_scr", [4, 128, LKP], BF16, kind="Internal").ap()
    v_scr = nc.dram_tensor("v_scr", [4, 128, NKB, 128], BF16, kind="Internal").ap()
    h1_scr = nc.dram_tensor("h1_scr", [NBLK * 128, D], F32, kind="Internal").ap()
    ct_scr = nc.dram_tensor("ct_scr", [128, 128, NBLK * 128], BF16, kind="Internal").ap()

    with ExitStack() as glob:
        P = Prog(nc, glob)

        def GT(name, shape, dt):
            return glob.enter_context(nc.sbuf_tensor("g_" + name, list(shape), dt))

        cst = GT("cst", [128, 640], BF16)
        spk = GT("spk", [128, C_END], F32)
        ident = cst[:, 0:128]
        NTi = cst[:, 128:256]
        NTl = cst[:, 256:384]
        onesm = cst[:, 384:512]
        NOnes = cst[:, 512:640]
        P.dma(cst[:], cst_d, writes=['cst'])
        P.dma(spk[:], spk_d, writes=['spk'])
        g1t = spk[:, C_G1:C_G1 + 8]
        g2t = spk[:, C_G2:C_G2 + 8]

        def rms_rows(ph, xin_ap, R, xn_ap, sst, xin_res, xn_res, junk, sst_res):
            P.op('act', lambda e: e.activation(out=junk[:R, :], in_=xin_ap, func=AF.Square,
                                               accum_out=sst[:R, 0:1]),
                 reads=[xin_res], writes=['junk', sst_res])
            P.op('act', lambda e: e.activation(out=sst[:R, 1:2], in_=sst[:R, 0:1], func=AF.Ln,
                                               bias=EPS, scale=1.0 / D),
                 reads=[sst_res], writes=[sst_res])
            P.op('act', lambda e: e.activation(out=sst[:R, 2:3], in_=sst[:R, 1:2], func=AF.Exp, scale=-0.5),
                 reads=[sst_res], writes=[sst_res])
            P.op('dve', lambda e: e.tensor_scalar(out=xn_ap, in0=xin_ap, scalar1=sst[:R, 2:3], scalar2=None,
                                                  op0=ALU.mult),
                 reads=[xin_res, sst_res], writes=[xn_res])

        class WPool:
            def __init__(self, stack, nbuf, nstage=2, tag="w"):
                self.wb = [stack.enter_context(nc.sbuf_tensor(f"{tag}b{i}", [128, 8, 512], BF16)) for i in range(nbuf)]
                self.ws = [stack.enter_context(nc.sbuf_tensor(f"{tag}s{i}", [128, 8, 512], F32)) for i in range(nstage)]
                self.tag = tag
                self.i = 0
                self.k = 0

            def load(self, src2d, kc=8, ncols=512, scale=None, kp=128):
                b = self.i % len(self.wb)
                self.i += 1
                s = self.k % len(self.ws)
                self.k += 1
                wb, ws = self.wb[b], self.ws[s]
                P.dma(ws[0:kp, 0:kc, 0:ncols], src2d.rearrange("(c p) n -> p c n", p=kp),
                      writes=[f'{self.tag}s{s}'])
                if scale is None:
                    P.op('pool', lambda e: e.tensor_copy(out=wb[0:kp, 0:kc, 0:ncols], in_=ws[0:kp, 0:kc, 0:ncols]),
                         reads=[f'{self.tag}s{s}'], writes=[f'{self.tag}b{b}'])
                else:
                    P.op('pool', lambda e: e.tensor_tensor(out=wb[0:kp, 0:kc, 0:ncols], in0=ws[0:kp, 0:kc, 0:ncols],
                                                           in1=scale.unsqueeze(2).to_broadcast([kp, kc, ncols]),
                                                           op=ALU.mult),
                         reads=[f'{self.tag}s{s}', 'spk'], writes=[f'{self.tag}b{b}'])
                return wb, f'{self.tag}b{b}'

        def dbg_dump(src_ap, dst_ap, res):
            P.dma(dst_ap, src_ap, reads=[res], writes=['dbg'])

        s_o = ExitStack()
        oT_all = s_o.enter_context(nc.sbuf_tensor("g_oT_all", [64, 8, NBLK * 128], BF16))
        s_q = ExitStack()
        qT_all = s_q.enter_context(nc.sbuf_tensor("g_qT_all", [128, 4, NBLK * 128], BF16))
        with ExitStack() as ph:
            def T(name, shape, dt):
                return ph.enter_context(nc.sbuf_tensor("p0_" + name, list(shape), dt))

            def PS(name, shape, dt):
                return ph.enter_context(nc.psum_tensor("ps0_" + name, list(shape), dt))

            wp = WPool(ph, 3, 2, tag="w1")
            wk, wk_r = wp.load(w_in[:, 1536:2048], scale=g1t)
            wv, wv_r = wp.load(w_in[:, 2048:2560], scale=g1t)
            wq, wq_r = wp.load(w_in[:, 1024:1536], scale=g1t)
            xin = [T(f"xin{i}", [128, D], F32) for i in range(2)]
            junk = T("junk", [128, D], BF16)
            sst = [T(f"sst{i}", [128, 4], F32) for i in range(2)]
            xn = [T(f"xn{i}", [128, D], BF16) for i in range(2)]
            hnT = [T(f"hnT{i}", [128, 8, 128], BF16) for i in range(2)]
            ksq = T("ksq", [128, 512], F32)
            kst = [T(f"kst{i}", [128, 24], F32) for i in range(2)]
            kn = [T(f"kn{i}", [128, 512], BF16) for i in range(2)]
            kTb = [T(f"kTb{i}", [128, 4, 128], BF16) for i in range(2)]
            vb = [T(f"vb{i}", [128, 512], BF16) for i in range(2)]
            ptr = [PS(f"ptr{i}", [128, 1024], BF16) for i in range(2)]
            kps = [PS(f"kps{i}", [128, 512], F32) for i in range(2)]
            vps = [PS(f"vps{i}", [128, 512], F32) for i in range(2)]
            pkt = PS("pkt", [128, 1024], BF16)
            DZ1 = PS("DZ1", [128, 512], F32) if N_WARM1 else None

            def front(i, src_rows, w_t, w_r, do_v, mid=None, stage='ab'):
                b = i % 2
                if 'a' in stage:
                    front_a(b, src_rows)
                if 'b' in stage:
                    front_b(b, w_t, w_r, do_v, mid)

            def front_a(b, src_rows):
                P.dma(xin[b][:], src_rows, writes=[f'xin{b}'])
                rms_rows(ph, xin[b][:], 128, xn[b][:], sst[b], f'xin{b}', f'xn{b}', junk, f'sst{b}')
                for c in range(8):
                    P.op('pe', lambda e, c=c: e.transpose(out=ptr[b][:, c * 128:(c + 1) * 128],
                                                          in_=xn[b][:, c * 128:(c + 1) * 128], identity=ident),
                         reads=[f'xn{b}', 'cst'], writes=[f'ptr{b}'])
                P.op('act', lambda e: e.activation(out=hnT[b][:].rearrange("p c n -> p (c n)"), in_=ptr[b][:],
                                                   func=AF.Copy),
                     reads=[f'ptr{b}'], writes=[f'hnT{b}'])

            def front_b(b, w_t, w_r, do_v, mid):
                if mid is not None:
                    mid()
                for c in range(8):
                    P.op('pe', lambda e, c=c: e.matmul(kps[b][:], lhsT=hnT[b][:, c, :], rhs=w_t[:, c, :],
                                                       start=(c == 0), stop=(c == 7)),
                         reads=[f'hnT{b}', w_r], writes=[f'kps{b}'])
                if do_v:
                    for c in range(8):
                        P.op('pe', lambda e, c=c: e.matmul(vps[b][:], lhsT=hnT[b][:, c, :], rhs=wv[:, c, :],
                                                           start=(c == 0), stop=(c == 7)),
                             reads=[f'hnT{b}', wv_r], writes=[f'vps{b}'])
                for _ in range(N_WARM1):
                    P.op('pe', lambda e: e.matmul(DZ1[:], lhsT=ident, rhs=wk[:, 0, :], start=True, stop=True),
                         reads=[wk_r, 'cst'], writes=['DZ1'])
                P.op('act', lambda e: e.activation(out=ksq[:], in_=kps[b][:], func=AF.Square),
                     reads=[f'kps{b}'], writes=['ksq'])
                P.op('dve', lambda e: e.tensor_reduce(out=kst[b][:, 0:8],
                                                      in_=ksq[:].rearrange("p (h d) -> p h d", h=8),
                                                      axis=AX.X, op=ALU.add),
                     reads=['ksq'], writes=[f'kst{b}'])
                P.op('act', lambda e: e.activation(out=kst[b][:, 8:16], in_=kst[b][:, 0:8], func=AF.Ln,
                                                   bias=EPS, scale=1.0 / 64),
                     reads=[f'kst{b}'], writes=[f'kst{b}'])
                P.op('act', lambda e: e.activation(out=kst[b][:, 16:24], in_=kst[b][:, 8:16], func=AF.Exp, scale=-0.5),
                     reads=[f'kst{b}'], writes=[f'kst{b}'])
                P.op('dve', lambda e: e.tensor_tensor(out=kn[b][:].rearrange("p (h d) -> p h d", h=8),
                                                      in0=kps[b][:].rearrange("p (h d) -> p h d", h=8),
                                                      in1=kst[b][:, 16:24].unsqueeze(2).to_broadcast([128, 8, 64]),
                                                      op=ALU.mult),
                     reads=[f'kps{b}', f'kst{b}'], writes=[f'kn{b}'])
                if do_v:
                    P.op('act', lambda e: e.activation(out=vb[b][:], in_=vps[b][:], func=AF.Copy),
                         reads=[f'vps{b}'], writes=[f'vb{b}'])

            def back(i, is_q, pb):
                b = i % 2
                for pr in range(4):
                    P.op('pe', lambda e, pr=pr: e.transpose(out=pkt[:, pr * 128:(pr + 1) * 128],
                                                            in_=kn[b][:, pr * 128:(pr + 1) * 128], identity=ident),
                         reads=[f'kn{b}', 'cst'], writes=['pkt'])
                if not is_q:
                    P.op('dve', lambda e: e.tensor_scalar(out=kTb[b][:].rearrange("p a n -> p (a n)"),
                                                          in0=pkt[:, 0:512], scalar1=spk[:, C_GK:C_GK + 1],
                                                          scalar2=None, op0=ALU.mult),
                         reads=['pkt', 'spk'], writes=[f'kTb{b}'])
                    P.dma(kT_scr[:, :, pb * 128:(pb + 1) * 128].rearrange("a p n -> p a n"), kTb[b][:],
                          reads=[f'kTb{b}'], writes=['kT_scr'])
                    P.dma(v_scr[:, :, pb, :].rearrange("a p n -> p a n"),
                          vb[b][:].rearrange("p (a n) -> p a n", a=4),
                          reads=[f'vb{b}'], writes=['v_scr'])
                else:
                    P.op('dve', lambda e: e.tensor_scalar(
                        out=qT_all[:, :, pb * 128:(pb + 1) * 128],
                        in0=pkt[:, 0:512].rearrange("p (a n) -> p a n", a=4),
                        scalar1=spk[:, C_GQ:C_GQ + 1], scalar2=0.125, op0=ALU.mult, op1=ALU.mult),
                         reads=['pkt', 'spk'], writes=['qT_all'])

            n1 = 0 if skip_kv else NKB
            items = [(False, pb) for pb in range(n1)] + [(True, blk) for blk in range(NBLK)]
            def item_args(is_q, idx):
                if is_q:
                    j, sb = idx // 4, idx % 4
                    return (xq[j, HALO + sb * 128:HALO + (sb + 1) * 128, :], wq, wq_r, False)
                return (hall[16 + idx * 128:16 + (idx + 1) * 128, :], wk, wk_r, True)

            prev = None
            front(0, *item_args(*items[0]), stage='a')
            for i, (is_q, idx) in enumerate(items):
                if i + 1 < len(items):
                    front(i + 1, *item_args(*items[i + 1]), stage='a')
                mid = (lambda pv=prev: back(*pv)) if prev is not None else None
                front(i, *item_args(is_q, idx), mid=mid, stage='b')
                prev = (i, is_q, idx)
            back(*prev)
            P.barrier()
            P.flush()

        if stop_after <= 1:
            with ExitStack() as ph:
                t = ph.enter_context(nc.sbuf_tensor("d1_dbt", [128, 2048], BF16))
                t32 = ph.enter_context(nc.sbuf_tensor("d1_dbt32", [128, 2048], F32))
                P.dma(t[:], kT_scr[0, :, 0:2048], writes=['dbt'])
                P.op('dve', lambda e: e.tensor_copy(out=t32[:], in_=t[:]), reads=['dbt'], writes=['dbt32'])
                P.dma(dbg_d[0], t32[:], reads=['dbt32'], writes=['dbg'])
                P.dma(t[:].rearrange("p (a n) -> p a n", a=16), v_scr[0, :, 0:16, :], writes=['dbt'])
                P.op('dve', lambda e: e.tensor_copy(out=t32[:], in_=t[:]), reads=['dbt'], writes=['dbt32'])
                P.dma(dbg_d[1], t32[:], reads=['dbt32'], writes=['dbg'])
                P.op('dve', lambda e: e.tensor_copy(out=t32[:], in_=qT_all[:, 0, :]), reads=['dbt', 'qT_all'],
                     writes=['dbt32'])
                P.dma(dbg_d[2], t32[:], reads=['dbt32'], writes=['dbg'])
                P.wait_all_dma()
                P.barrier()
                P.flush()
            s_q.close()
            s_o.close()
            return nc

        with ExitStack() as ph:
            def T(name, shape, dt):
                return ph.enter_context(nc.sbuf_tensor("p1_" + name, list(shape), dt))

            def PS(name, shape, dt):
                return ph.enter_context(nc.psum_tensor("ps1_" + name, list(shape), dt))

            amask = T("amask", [128, 33 * 512], BF16)
            P.dma(amask[:], amask_d, writes=['amask'])
            kTp = T("kTp", [128, LKP], BF16)
            vp = T("vp", [128, NKB, 128], BF16)
            NE1, NSP, NER, NW, NZ = 6, 5, 3, 3, 3
            e1 = [T(f"e1_{i}", [128, 512], F32) for i in range(NE1)]
            spb = [T(f"sp_{i}", [128, 512], BF16) for i in range(NSP)]
            er = [T(f"er_{i}", [128, 512], F32) for i in range(NER)]
            wt = [T(f"wt_{i}", [128, 512], BF16) for i in range(NW)]
            CS = [[T(f"cs{s_}_{i}", [128, 512], BF16) for i in range(2)] for s_ in range(2)]
            Z = [PS(f"Z{i}", [128, 512], F32) for i in range(NZ)]
            RA = [PS(f"RA{i}", [128, 512], F32) for i in range(2)]
            OT = [PS(f"OT{i}", [128, 512], F32) for i in range(2)]
            DZ = PS("DZ", [128, 512], F32) if N_WARM else None

            if att_pairs < 4 or att_slots < 4:
                P.op('dve', lambda e: e.memset(oT_all[:], 0.0), writes=['oT_all'])
            KCH = (0, 33, 65, 97, NKB)

            def kch(kb):
                return 0 if kb < 33 else 1 + (kb - 33) // 32

            for pr in range(att_pairs):
                for ch in (3, 2, 1, 0):
                    k0, k1 = KCH[ch], KCH[ch + 1]
                    P.dma(kTp[:, k0 * 128:k1 * 128], kT_scr[pr, :, k0 * 128:k1 * 128], reads=['kT_scr'],
                          writes=[f'kTp{ch}'])
                    P.dma(vp[:, k0:k1, :], v_scr[pr, :, k0:k1, :], reads=['v_scr'], writes=[f'vp{ch}'])
                for j in range(att_slots):
                    nkb = 32 * j + 33
                    tiles = []
                    for kk in range(nkb):
                        kb = 32 * j + 32 - kk
                        for s in range(2):
                            tiles.append((s, kb, kk))
                    NT = len(tiles)
                    qcols = slice(j * 512, (j + 1) * 512)

                    def QK(t):
                        s, kb, kk = tiles[t]
                        ho = s * 64
                        P.op('pe', lambda e: e.matmul(Z[t % NZ][:], lhsT=kTp[ho:ho + 64, kb * 128:(kb + 1) * 128],
                                                      rhs=qT_all[ho:ho + 64, pr, qcols], start=True, stop=True),
                             reads=[f'kTp{kch(kb)}', 'qT_all'], writes=[f'Z{t % NZ}'])

                    def EXP1(t):
                        s, kb, kk = tiles[t]
                        P.op('act', lambda e: e.activation(out=e1[t % NE1][:], in_=Z[t % NZ][:], func=AF.Exp),
                             reads=[f'Z{t % NZ}'], writes=[f'e1_{t % NE1}'])
                        r = kb - 32 * j
                        if 0 <= r <= 32:
                            P.op('dve', lambda e: e.tensor_tensor(out=e1[t % NE1][:], in0=e1[t % NE1][:],
                                                                  in1=amask[:, r * 512:(r + 1) * 512], op=ALU.mult),
                                 reads=[f'e1_{t % NE1}', 'amask'], writes=[f'e1_{t % NE1}'])

                    def LN(t):
                        P.op('act', lambda e: e.activation(out=spb[t % NSP][:], in_=e1[t % NE1][:], func=AF.Ln,
                                                           bias=1.0, scale=1.0),
                             reads=[f'e1_{t % NE1}'], writes=[f'sp_{t % NSP}'])

                    def TRI(t):
                        s, kb, kk = tiles[t]
                        P.op('pe', lambda e: e.matmul(RA[s][:], lhsT=NTi, rhs=spb[t % NSP][:],
                                                      start=True, stop=(kk == 0)),
                             reads=[f'sp_{t % NSP}', 'cst'], writes=[f'RA{s}'])
                        if kk > 0:
                            P.op('pe', lambda e: e.matmul(RA[s][:], lhsT=NOnes, rhs=CS[s][kk % 2][:],
                                                          start=False, stop=True),
                                 reads=[f'cs{s}_{kk % 2}', 'cst'], writes=[f'RA{s}'])
                        if kk < nkb - 1:
                            if kk == 0:
                                P.op('dve', lambda e: e.tensor_copy(out=CS[s][1][:], in_=spb[t % NSP][:]),
                                     reads=[f'sp_{t % NSP}'], writes=[f'cs{s}_1'])
                            else:
                                P.op('dve', lambda e: e.tensor_tensor(out=CS[s][(kk + 1) % 2][:],
                                                                       in0=CS[s][kk % 2][:], in1=spb[t % NSP][:],
                                                                       op=ALU.add),
                                     reads=[f'cs{s}_{kk % 2}', f'sp_{t % NSP}'], writes=[f'cs{s}_{(kk + 1) % 2}'])

                    def EXPR(t):
                        s, kb, kk = tiles[t]
                        P.op('act', lambda e: e.activation(out=er[t % NER][:], in_=RA[s][:], func=AF.Exp),
                             reads=[f'RA{s}'], writes=[f'er_{t % NER}'])

                    def WMUL(t):
                        P.op('dve', lambda e: e.tensor_tensor(out=wt[t % NW][:], in0=e1[t % NE1][:],
                                                              in1=er[t % NER][:], op=ALU.mult),
                             reads=[f'e1_{t % NE1}', f'er_{t % NER}'], writes=[f'wt_{t % NW}'])

                    def AV(t):
                        s, kb, kk = tiles[t]
                        ho = s * 64
                        P.op('pe', lambda e: e.matmul(OT[s][0:64, :], lhsT=vp[:, kb, ho:ho + 64], rhs=wt[t % NW][:],
                                                      start=(kk == 0), stop=(kk == nkb - 1)),
                             reads=[f'vp{kch(kb)}', f'wt_{t % NW}'], writes=[f'OT{s}'])
                        if kk == nkb - 1:
                            h = pr * 2 + s
                            P.op('act', lambda e: e.activation(out=oT_all[:, h, qcols], in_=OT[s][0:64, :],
                                                               func=AF.Copy),
                                 reads=[f'OT{s}'], writes=['oT_all'])

                    for n in range(-2, NT + 2):
                        if 0 <= n + 2 < NT:
                            QK(n + 2)
                        if 0 <= n + 1 < NT:
                            EXP1(n + 1)
                        if 0 <= n < NT:
                            LN(n)
                            TRI(n)
                        if 0 <= n - 1 < NT:
                            EXPR(n - 1)
                            WMUL(n - 1)
                        if 0 <= n - 2 < NT:
                            AV(n - 2)
                        for _ in range(N_WARM):
                            P.op('pe', lambda e: e.matmul(DZ[:], lhsT=NTi, rhs=cst[:, 0:512], start=True, stop=True),
                                 reads=['cst'], writes=['DZ'])
            P.barrier()
            P.flush()

        s_q.close()
        if stop_after <= 3:
            with ExitStack() as ph:
                t32 = ph.enter_context(nc.sbuf_tensor("d3_dbt32", [64, 8, 2048], F32))
                P.op('dve', lambda e: e.tensor_copy(out=t32[:], in_=oT_all[:]), reads=['oT_all'], writes=['dbt32'])
                P.dma(dbg_d, t32[:], reads=['dbt32'], writes=['dbg'])
                P.wait_all_dma()
                P.barrier()
                P.flush()
            s_o.close()
            return nc

        with ExitStack() as ph:
            def T(name, shape, dt):
                return ph.enter_context(nc.sbuf_tensor("p2_" + name, list(shape), dt))

            def PS(name, shape, dt):
                return ph.enter_context(nc.psum_tensor("ps2_" + name, list(shape), dt))

            wp = WPool(ph, 3, 2, tag="w4")
            hres = T("hres", [128, 4, D], F32)
            xh = T("xh", [HALO, D], F32)
            junk = T("junk", [128, D], BF16)
            sst = [T(f"sst{i}", [128, 4], F32) for i in range(2)]
            xn = [T(f"xn{i}", [128, D], BF16) for i in range(2)]
            hnTs = T("hnTs", [128, 8, SLOT + HALO], BF16)
            glu = T("glu", [128, 4, SLOT + HALO], F32)
            acc = T("acc", [128, 4, SLOT], F32)
            hb = T("hb", [128, 4, SLOT], BF16)
            hsq = T("hsq", [128, 4, SLOT], BF16)
            ycT = T("ycT", [128, 4, SLOT], BF16)
            mix = T("mix", [128, 8, SLOT], F32)
            mixT = T("mixT", [128, 8, SLOT], BF16)
            tmp = [T(f"tmp{i}", [128, SLOT], F32) for i in range(4)]
            lnst = [T(f"lnst{i}", [128, SLOT], F32) for i in range(3)]
            ptr = [PS(f"ptr{i}", [128, 1024], BF16) for i in range(2)]
            psA = [PS(f"psA{i}", [128, 512], F32) for i in range(2)]
            psG = [PS(f"psG{i}", [128, 512], F32) for i in range(2)]
            psH = PS("psH", [128, 512], F32)
            psM = PS("psM", [128, 512], F32)

            for j in range(NSLOT):
                P.dma(xh[:], xq[j, 0:HALO, :], writes=['xh'])
                P.dma(hres[:], xq[j, HALO:HALO + SLOT, :].rearrange("(s p) n -> p s n", p=128), writes=['hres'])
                for sb in range(-1, 4):
                    b = (sb + 1) % 2
                    if sb < 0:
                        R, xin_ap, xres, col0 = HALO, xh[:], 'xh', 0
                    else:
                        R, xin_ap, xres, col0 = 128, hres[:, sb, :], 'hres', HALO + sb * 128
                    rms_rows(ph, xin_ap, R, xn[b][:R, :], sst[b], xres, f'xn{b}', junk, f'sst{b}')
                    for c in range(8):
                        P.op('pe', lambda e, c=c, R=R, b=b: e.transpose(out=ptr[b][:, c * 128:c * 128 + R],
                                                                        in_=xn[b][:R, c * 128:(c + 1) * 128],
                                                                        identity=cst[:R, 0:R]),
                             reads=[f'xn{b}', 'cst'], writes=[f'ptr{b}'])
                    P.op('act', lambda e, R=R, b=b, col0=col0: e.activation(
                        out=hnTs[:, :, col0:col0 + R],
                        in_=ptr[b][:].rearrange("p (c n) -> p c n", c=8)[:, :, 0:R], func=AF.Copy),
                         reads=[f'ptr{b}'], writes=['hnTs'])
                if p4_stop <= 1:
                    P.dma(h1_scr[j * 512:(j + 1) * 512, :].rearrange("(s p) n -> p s n", p=128), hres[:],
                          reads=['hres'], writes=['h1_scr'])
                    continue
                wa, wa_r = wp.load(w_in[:, 0:512], scale=g1t)
                wg_, wg_r = wp.load(w_in[:, 512:1024], scale=g1t)
                for cc in range(4):
                    pb_ = cc % 2
                    for (wt_, wr_, pst, hcol) in ((wa, wa_r, psA, 0), (wg_, wg_r, psG, 32)):
                        for c in range(8):
                            P.op('pe', lambda e, c=c, wt_=wt_, pst=pst: e.matmul(
                                pst[pb_][:], lhsT=wt_[:, c, cc * 128:(cc + 1) * 128], rhs=hnTs[:, c, HALO:HALO + SLOT],
                                start=(c == 0), stop=(c == 7)),
                                 reads=[wr_, 'hnTs'], writes=[f'{"psA" if pst is psA else "psG"}{pb_}'])
                        for c in range(8):
                            P.op('pe', lambda e, c=c, wt_=wt_, hcol=hcol: e.matmul(
                                psH[:, cc * 64 + hcol:cc * 64 + hcol + 32], lhsT=wt_[:, c, cc * 128:(cc + 1) * 128],
                                rhs=hnTs[:, c, 0:HALO], start=(c == 0), stop=(c == 7)),
                                 reads=[wr_, 'hnTs'], writes=['psH'])
                    tb = tmp[cc % 2]
                    P.op('act', lambda e, tb=tb: e.activation(out=tb[:], in_=psG[pb_][:], func=AF.Sigmoid),
                         reads=[f'psG{pb_}'], writes=[f'tmp{cc % 2}'])
                    P.op('dve', lambda e, tb=tb: e.tensor_tensor(out=glu[:, cc, HALO:HALO + SLOT], in0=psA[pb_][:],
                                                                 in1=tb[:], op=ALU.mult),
                         reads=[f'psA{pb_}', f'tmp{cc % 2}'], writes=[f'glu{cc}'])
                    tb2 = tmp[2 + cc % 2]
                    P.op('act', lambda e, tb2=tb2: e.activation(out=tb2[:, 0:32], in_=psH[:, cc * 64 + 32:cc * 64 + 64],
                                                                func=AF.Sigmoid),
                         reads=['psH'], writes=[f'tmp{2 + cc % 2}'])
                    P.op('dve', lambda e, tb2=tb2: e.tensor_tensor(out=glu[:, cc, 0:HALO], in0=psH[:, cc * 64:cc * 64 + 32],
                                                                   in1=tb2[:, 0:32], op=ALU.mult),
                         reads=['psH', f'tmp{2 + cc % 2}'], writes=[f'glu{cc}'])
                if p4_stop <= 2:
                    P.dma(h1_scr[j * 512:(j + 1) * 512, :].rearrange("(s p) n -> p s n", p=128), hres[:],
                          reads=['hres'], writes=['h1_scr'])
                    continue
                for cc in range(4):
                    eng = 'dve'
                    cw0 = C_CW + cc * 31
                    P.op(eng, lambda e, cw0=cw0: e.tensor_scalar(out=acc[:, cc, :], in0=glu[:, cc, 2:2 + SLOT],
                                                                 scalar1=spk[:, cw0:cw0 + 1],
                                                                 scalar2=spk[:, C_CB + cc:C_CB + cc + 1],
                                                                 op0=ALU.mult, op1=ALU.add),
                         reads=[f'glu{cc}', 'spk'], writes=[f'acc{cc}'])
                    for w in range(1, 31):
                        P.op(eng, lambda e, cw0=cw0, w=w: e.scalar_tensor_tensor(
                            out=acc[:, cc, :], in0=glu[:, cc, 2 + w:2 + w + SLOT], scalar=spk[:, cw0 + w:cw0 + w + 1],
                            in1=acc[:, cc, :], op0=ALU.mult, op1=ALU.add),
                             reads=[f'glu{cc}', 'spk', f'acc{cc}'], writes=[f'acc{cc}'])
                if p4_stop <= 3:
                    P.dma(h1_scr[j * 512:(j + 1) * 512, :].rearrange("(s p) n -> p s n", p=128), hres[:],
                          reads=['hres'], writes=['h1_scr'])
                    continue
                for cc in range(4):
                    P.op('act', lambda e: e.activation(out=hb[:, cc, :], in_=acc[:, cc, :], func=AF.Copy),
                         reads=[f'acc{cc}'], writes=[f'hb{cc}'])
                    P.op('act', lambda e: e.activation(out=hsq[:, cc, :], in_=acc[:, cc, :], func=AF.Square),
                         reads=[f'acc{cc}'], writes=[f'hsq{cc}'])
                for cc in range(4):
                    P.op('pe', lambda e: e.matmul(psM[:], lhsT=onesm, rhs=hb[:, cc, :], start=(cc == 0), stop=(cc == 3)),
                         reads=[f'hb{cc}', 'cst'], writes=['psM'])
                for cc in range(4):
                    P.op('pe', lambda e: e.matmul(psH[:], lhsT=onesm, rhs=hsq[:, cc, :], start=(cc == 0), stop=(cc == 3)),
                         reads=[f'hsq{cc}', 'cst'], writes=['psH'])
                P.op('act', lambda e: e.activation(out=lnst[0][:], in_=psM[:], func=AF.Copy),
                     reads=['psM'], writes=['lnst0'])
                P.op('act', lambda e: e.activation(out=lnst[1][:], in_=psM[:], func=AF.Square),
                     reads=['psM'], writes=['lnst1'])
                P.op('dve', lambda e: e.tensor_tensor(out=lnst[1][:], in0=psH[:], in1=lnst[1][:], op=ALU.subtract),
                     reads=['psH', 'lnst1'], writes=['lnst1'])
                P.op('act', lambda e: e.activation(out=lnst[2][:], in_=lnst[1][:], func=AF.Ln, bias=EPS, scale=1.0),
                     reads=['lnst1'], writes=['lnst2'])
                P.op('act', lambda e: e.activation(out=lnst[2][:], in_=lnst[2][:], func=AF.Exp, scale=-0.5),
                     reads=['lnst2'], writes=['lnst2'])
                for cc in range(4):
                    tb = tmp[cc % 2]
                    P.op('dve', lambda e, tb=tb: e.tensor_tensor(out=tb[:], in0=acc[:, cc, :], in1=lnst[0][:],
                                                                 op=ALU.subtract),
                         reads=[f'acc{cc}', 'lnst0'], writes=[f'tmp{cc % 2}'])
                    P.op('dve', lambda e, tb=tb: e.tensor_tensor(out=tb[:], in0=tb[:], in1=lnst[2][:], op=ALU.mult),
                         reads=[f'tmp{cc % 2}', 'lnst2'], writes=[f'tmp{cc % 2}'])
                    P.op('act', lambda e, tb=tb: e.activation(out=ycT[:, cc, :], in_=tb[:], func=AF.Silu,
                                                              bias=spk[:, C_LB + cc:C_LB + cc + 1],
                                                              scale=spk[:, C_LG + cc:C_LG + cc + 1]),
                         reads=[f'tmp{cc % 2}', 'spk'], writes=[f'ycT{cc}'])
                if p4_stop <= 4:
                    P.dma(h1_scr[j * 512:(j + 1) * 512, :].rearrange("(s p) n -> p s n", p=128), hres[:],
                          reads=['hres'], writes=['h1_scr'])
                    continue
                for part in range(2):
                    for half in range(2):
                        wgt, wgt_r = wp.load(w_gate[:, part * 1024 + half * 512:part * 1024 + (half + 1) * 512],
                                             scale=g1t)
                        if part == 0:
                            wy, wy_r = wp.load(w_conv_out[:, half * 512:(half + 1) * 512], kc=4)
                        else:
                            wy, wy_r = wp.load(w_sb_out[:, half * 512:(half + 1) * 512], kc=8, kp=64)
                        for mm in range(4):
                            m = half * 4 + mm
                            pb_ = m % 2
                            for c in range(8):
                                P.op('pe', lambda e, c=c: e.matmul(psG[pb_][:], lhsT=wgt[:, c, mm * 128:(mm + 1) * 128],
                                                                   rhs=hnTs[:, c, HALO:HALO + SLOT],
                                                                   start=(c == 0), stop=(c == 7)),
                                     reads=[wgt_r, 'hnTs'], writes=[f'psG{pb_}'])
                            if part == 0:
                                for cc in range(4):
                                    P.op('pe', lambda e, cc=cc: e.matmul(psA[pb_][:],
                                                                         lhsT=wy[:, cc, mm * 128:(mm + 1) * 128],
                                                                         rhs=ycT[:, cc, :], start=(cc == 0), stop=(cc == 3)),
                                         reads=[wy_r, f'ycT{cc}'], writes=[f'psA{pb_}'])
                            else:
                                for h in range(8):
                                    P.op('pe', lambda e, h=h: e.matmul(psA[pb_][:],
                                                                       lhsT=wy[0:64, h, mm * 128:(mm + 1) * 128],
                                                                       rhs=oT_all[:, h, j * 512:(j + 1) * 512],
                                                                       start=(h == 0), stop=(h == 7)),
                                         reads=[wy_r, 'oT_all'], writes=[f'psA{pb_}'])
                            tb = tmp[m % 2]
                            bcol = C_BG + part * 8 + m
                            P.op('act', lambda e, tb=tb, bcol=bcol: e.activation(out=tb[:], in_=psG[pb_][:],
                                                                                 func=AF.Sigmoid,
                                                                                 bias=spk[:, bcol:bcol + 1], scale=1.0),
                                 reads=[f'psG{pb_}', 'spk'], writes=[f'tmp{m % 2}'])
                            if part == 0:
                                P.op('dve', lambda e, tb=tb: e.tensor_tensor(out=mix[:, m, :], in0=psA[pb_][:], in1=tb[:],
                                                                             op=ALU.mult),
                                     reads=[f'psA{pb_}', f'tmp{m % 2}'], writes=[f'mix{m}'])
                            else:
                                P.op('dve', lambda e, tb=tb: e.tensor_tensor(out=tb[:], in0=psA[pb_][:], in1=tb[:],
                                                                             op=ALU.mult),
                                     reads=[f'psA{pb_}', f'tmp{m % 2}'], writes=[f'tmp{m % 2}'])
                                P.op('pool', lambda e, tb=tb: e.tensor_tensor(out=mixT[:, m, :], in0=mix[:, m, :],
                                                                              in1=tb[:], op=ALU.add),
                                     reads=[f'mix{m}', f'tmp{m % 2}'], writes=[f'mixT{m}'])
                if p4_stop <= 5:
                    P.dma(h1_scr[j * 512:(j + 1) * 512, :].rearrange("(s p) n -> p s n", p=128), hres[:],
                          reads=['hres'], writes=['h1_scr'])
                    continue
                for half in range(2):
                    wo, wo_r = wp.load(w_o[:, half * 512:(half + 1) * 512])
                    for sb in range(4):
                        pb_ = sb % 2
                        for m in range(8):
                            P.op('pe', lambda e, m=m: e.matmul(psA[pb_][:], lhsT=mixT[:, m, sb * 128:(sb + 1) * 128],
                                                               rhs=wo[:, m, :], start=(m == 0), stop=(m == 7)),
                                 reads=[wo_r, f'mixT{m}'], writes=[f'psA{pb_}'])
                        P.op('dve', lambda e: e.tensor_tensor(out=hres[:, sb, half * 512:(half + 1) * 512],
                                                              in0=psA[pb_][:],
                                                              in1=hres[:, sb, half * 512:(half + 1) * 512], op=ALU.add),
                             reads=[f'psA{pb_}', 'hres'], writes=['hres'])
                P.dma(h1_scr[j * 512:(j + 1) * 512, :].rearrange("(s p) n -> p s n", p=128), hres[:],
                      reads=['hres'], writes=['h1_scr'])
            P.wait_all_dma()
            P.barrier()
            P.flush()

        s_o.close()
        if stop_after <= 4:
            with ExitStack() as ph:
                t32 = ph.enter_context(nc.sbuf_tensor("d4_dbt32", [128, NBLK, D], F32))
                P.dma(t32[:], h1_scr.rearrange("(s p) n -> p s n", p=128), reads=['h1_scr'], writes=['dbt32'])
                P.dma(out_d.rearrange("(s p) n -> p s n", p=128), t32[:], reads=['dbt32'], writes=['out'])
                P.wait_all_dma()
                P.barrier()
                P.flush()
            return nc

        hn2T = GT("hn2T", [128, 8, NBLK * 128], BF16)
        with ExitStack() as ph:
            def T(name, shape, dt):
                return ph.enter_context(nc.sbuf_tensor("p3_" + name, list(shape), dt))

            def PS(name, shape, dt):
                return ph.enter_context(nc.psum_tensor("ps3_" + name, list(shape), dt))

            wp = WPool(ph, 2, 1, tag="w5")
            kkT = T("kkT", [128, 16, 128], BF16)
            qpT = T("qpT", [128, 16, SLOT], BF16)
            S = T("S", [128, 16, 128], F32)
            wk1 = T("wk1", [128, 256], F32)
            V16 = T("V16", [128, 16, 16], F32)
            cand = T("cand", [128, 2, 256], F32)
            best = T("best", [128, 8, 16], F32)
            st5 = T("st5", [128, 64], F32)
            junk16 = T("junk16", [128, 16], F32)
            E12 = T("E12", [128, 16, 128], F32)
            psq = [PS(f"psq{i}", [128, 512], F32) for i in range(1)] * 2
            pss = [PS(f"pss{i}", [128, 512], F32) for i in range(1)] * 2

            s_k = ExitStack()
            kkf = s_k.enter_context(nc.sbuf_tensor("p5_kkf", [128, 16, 128], F32))
            kkb = s_k.enter_context(nc.sbuf_tensor("p5_kkb", [128, 16, 128], BF16))
            xin = [s_k.enter_context(nc.sbuf_tensor(f"p5_xin{i}", [128, D], F32)) for i in range(2)]
            junk = s_k.enter_context(nc.sbuf_tensor("p5_junk", [128, D], BF16))
            sst = [s_k.enter_context(nc.sbuf_tensor(f"p5_sst{i}", [128, 4], F32)) for i in range(2)]
            xn = [s_k.enter_context(nc.sbuf_tensor(f"p5_xn{i}", [128, D], BF16)) for i in range(2)]
            ptr = [s_k.enter_context(nc.psum_tensor(f"ps5_ptr{i}", [128, 1024], BF16)) for i in range(2)]
            P.dma(kkf[:], peer_k.rearrange("q k d -> k q d"), writes=['kkf'])
            P.op('dve', lambda e: e.tensor_copy(out=kkb[:], in_=kkf[:]), reads=['kkf'], writes=['kkb'])
            for qi in range(16):
                b = (qi // 8) % 2
                P.op('pe', lambda e, qi=qi, b=b: e.transpose(out=ptr[b][:, (qi % 8) * 128:(qi % 8 + 1) * 128],
                                                             in_=kkb[:, qi, :], identity=ident),
                     reads=['kkb', 'cst'], writes=[f'ptr{b}'])
                if qi % 8 == 7:
                    P.op('act', lambda e, qi=qi, b=b: e.activation(
                        out=kkT[:, qi - 7:qi + 1, :].rearrange("p a n -> p (a n)"), in_=ptr[b][:], func=AF.Copy),
                         reads=[f'ptr{b}'], writes=['kkT'])
            for blk in range(NBLK):
                b = blk % 2
                P.dma(xin[b][:], h1_scr[blk * 128:(blk + 1) * 128, :], reads=['h1_scr'], writes=[f'xin{b}'])
                rms_rows(ph, xin[b][:], 128, xn[b][:], sst[b], f'xin{b}', f'xn{b}', junk, f'sst{b}')
                for c in range(8):
                    P.op('pe', lambda e, c=c, b=b: e.transpose(out=ptr[b][:, c * 128:(c + 1) * 128],
                                                               in_=xn[b][:, c * 128:(c + 1) * 128], identity=ident),
                         reads=[f'xn{b}', 'cst'], writes=[f'ptr{b}'])
                P.op('act', lambda e, b=b, blk=blk: e.activation(
                    out=hn2T[:, :, blk * 128:(blk + 1) * 128],
                    in_=ptr[b][:].rearrange("p (c n) -> p c n", c=8), func=AF.Copy),
                     reads=[f'ptr{b}'], writes=['hn2T'])
            P.barrier()
            P.flush()
            s_k.close()
            ebuf = [T(f"ebuf{i}", [128, 2048], F32) for i in range(2)]
            ebufA = [[T(f"ebufA{i}_{a}", [128, 2048], F32) for a in range(len(ACT_HEADS))] for i in range(2)]
            csum = T("csum", [128, 2048], BF16)
            NCM = 6
            cm = [T(f"cm{i}", [128, 2048], BF16) for i in range(NCM)]
            ctb = [T(f"ctb{i}", [128, 16, 128], BF16) for i in range(2)]
            pacc = [PS(f"pacc{i}", [128, 512], F32) for i in range(4)]
            pct = PS("pct", [128, 2048], BF16)

            for tg in range(4):
                for piece in range(4):
                    wqp, wqp_r = wp.load(peer_wq[:, piece * 512:(piece + 1) * 512], scale=g2t)
                    for qq in range(4):
                        qi = piece * 4 + qq
                        b = qi % 2
                        for c in range(8):
                            P.op('pe', lambda e, c=c, qq=qq, b=b: e.matmul(
                                psq[b][:], lhsT=wqp[:, c, qq * 128:(qq + 1) * 128],
                                rhs=hn2T[:, c, tg * 512:(tg + 1) * 512], start=(c == 0), stop=(c == 7)),
                                 reads=[wqp_r, 'hn2T'], writes=['psq0'])
                        P.op('act', lambda e, qi=qi, b=b: e.activation(out=qpT[:, qi, :], in_=psq[b][:], func=AF.Copy),
                             reads=['psq0'], writes=[f'qpT{qi}'])
                for bb in range(4):
                    blk = tg * 4 + bb
                    for g4 in range(4):
                        b = g4 % 2
                        for qq in range(4):
                            qi = g4 * 4 + qq
                            P.op('pe', lambda e, qi=qi, qq=qq, b=b: e.matmul(
                                pss[b][:, qq * 128:(qq + 1) * 128], lhsT=qpT[:, qi, bb * 128:(bb + 1) * 128],
                                rhs=kkT[:, qi, :], start=True, stop=True),
                                 reads=[f'qpT{qi}', 'kkT'], writes=['pss0'])
                        P.op('act', lambda e, g4=g4, b=b: e.activation(
                            out=S[:, g4 * 4:(g4 + 1) * 4, :].rearrange("p a n -> p (a n)"), in_=pss[b][:], func=AF.Copy),
                             reads=['pss0'], writes=['S'])
                    for qi in range(16):
                        P.op('dve', lambda e, qi=qi: e.max(out=V16[:, qi, 0:8], in_=S[:, qi, :]),
                             reads=['S'], writes=['V16'])
                        P.op('dve', lambda e, qi=qi: e.match_replace(out=wk1[:, 0:128], in_to_replace=V16[:, qi, 0:8],
                                                                     in_values=S[:, qi, :], imm_value=NEG),
                             reads=['S', 'V16'], writes=['wk1'])
                        P.op('dve', lambda e, qi=qi: e.max(out=V16[:, qi, 8:16], in_=wk1[:, 0:128]),
                             reads=['wk1'], writes=['V16'])
                    for h in range(8):
                        P.op('dve', lambda e, h=h: e.tensor_tensor(
                            out=cand[:, h % 2, :].rearrange("p (a b) -> p a b", a=16),
                            in0=V16[:, 2 * h, :].unsqueeze(2).to_broadcast([128, 16, 16]),
                            in1=V16[:, 2 * h + 1, :].unsqueeze(1).to_broadcast([128, 16, 16]), op=ALU.add),
                             reads=['V16'], writes=[f'cand{h % 2}'])
                        P.op('dve', lambda e, h=h: e.max(out=best[:, h, 0:8], in_=cand[:, h % 2, :]),
                             reads=[f'cand{h % 2}'], writes=['best'])
                        P.op('dve', lambda e, h=h: e.match_replace(out=wk1[:], in_to_replace=best[:, h, 0:8],
                                                                   in_values=cand[:, h % 2, :], imm_value=NEG),
                             reads=[f'cand{h % 2}', 'best'], writes=['wk1'])
                        P.op('dve', lambda e, h=h: e.max(out=best[:, h, 8:16], in_=wk1[:]),
                             reads=['wk1'], writes=['best'])
                    V4 = V16[:].rearrange("p (h t) k -> p h t k", t=2)
                    P.op('dve', lambda e: e.tensor_scalar(out=st5[:, 0:8], in0=best[:, :, 0], scalar1=-1.0, scalar2=None,
                                                          op0=ALU.mult),
                         reads=['best'], writes=['st5a'])
                    for h in range(8):
                        P.op('act', lambda e, h=h: e.activation(out=junk16[:], in_=best[:, h, :], func=AF.Exp,
                                                                bias=st5[:, h:h + 1], scale=1.0,
                                                                accum_out=st5[:, 8 + h:9 + h]),
                             reads=['best', 'st5a'], writes=['junk16', 'st5b'])
                    P.op('act', lambda e: e.activation(out=st5[:, 16:24], in_=st5[:, 8:16], func=AF.Ln),
                         reads=['st5b'], writes=['st5c'])
                    P.op('dve', lambda e: e.tensor_scalar(out=st5[:, 24:32], in0=V4[:, :, 0, 0], scalar1=-1.0,
                                                          scalar2=None, op0=ALU.mult),
                         reads=['V16'], writes=['st5d'])
                    P.op('dve', lambda e: e.scalar_tensor_tensor(out=st5[:, 32:40], in0=V4[:, :, 1, 0], scalar=-1.0,
                                                                 in1=st5[:, 16:24], op0=ALU.mult, op1=ALU.subtract),
                         reads=['V16', 'st5c'], writes=['st5e'])
                    P.op('dve', lambda e: e.tensor_tensor(out=st5[:, 40:48], in0=best[:, :, 15], in1=st5[:, 0:8],
                                                          op=ALU.add),
                         reads=['best', 'st5a'], writes=['st5f'])
                    P.op('dve', lambda e: e.tensor_tensor(out=st5[:, 40:48], in0=st5[:, 40:48], in1=st5[:, 16:24],
                                                          op=ALU.subtract),
                         reads=['st5f', 'st5c'], writes=['st5f'])
                    P.op('act', lambda e: e.activation(out=st5[:, 48:56], in_=st5[:, 40:48], func=AF.Exp),
                         reads=['st5f'], writes=['st5g'])
                    P.op('dve', lambda e: e.tensor_scalar(out=st5[:, 48:56], in0=st5[:, 48:56], scalar1=1.0 - 1e-4,
                                                          scalar2=None, op0=ALU.mult),
                         reads=['st5g'], writes=['st5g'])
                    for h in range(8):
                        P.op('act', lambda e, h=h: e.activation(out=E12[:, 2 * h, :], in_=S[:, 2 * h, :], func=AF.Exp,
                                                                bias=st5[:, 24 + h:25 + h], scale=1.0),
                             reads=['S', 'st5d'], writes=['E12'])
                        P.op('act', lambda e, h=h: e.activation(out=E12[:, 2 * h + 1, :], in_=S[:, 2 * h + 1, :],
                                                                func=AF.Exp, bias=st5[:, 32 + h:33 + h], scale=1.0),
                             reads=['S', 'st5e'], writes=['E12'])
                    def act_outer(q8_):
                        for ai, h in enumerate(ACT_HEADS):
                            eb = ebufA[q8_ % 2][ai]
                            for il in range(16):
                                i1c = q8_ * 16 + il
                                P.op('act', lambda e, h=h, il=il, i1c=i1c, eb=eb: e.activation(
                                    out=eb[:, il * 128:(il + 1) * 128], in_=E12[:, 2 * h + 1, :], func=AF.Copy,
                                    scale=E12[:, 2 * h, i1c:i1c + 1]),
                                     reads=['E12'], writes=[f'ebufA{q8_ % 2}_{ai}'])

                    act_outer(0)
                    for q8 in range(8):
                        cs_ = q8 % 2
                        if q8 + 1 < 8:
                            act_outer(q8 + 1)
                        for hi, h in enumerate(HEAD_ORDER):
                            kc_ = (q8 * 8 + hi) % NCM
                            if h in ACT_HEADS:
                                ai = ACT_HEADS.index(h)
                                src, sres = ebufA[cs_][ai], f'ebufA{cs_}_{ai}'
                            else:
                                k_ = (q8 * 8 + hi) % 2
                                src, sres = ebuf[k_], f'ebuf{k_}'
                                P.op('dve', lambda e, h=h, src=src: e.tensor_tensor(
                                    out=src[:].rearrange("p (a b) -> p a b", a=16),
                                    in0=E12[:, 2 * h, q8 * 16:(q8 + 1) * 16].unsqueeze(2).to_broadcast([128, 16, 128]),
                                    in1=E12[:, 2 * h + 1, :].unsqueeze(1).to_broadcast([128, 16, 128]), op=ALU.mult),
                                     reads=['E12'], writes=[sres])
                            P.op('dve', lambda e, h=h, src=src, kc_=kc_: e.scalar_tensor_tensor(
                                out=cm[kc_][:], in0=src[:], scalar=st5[:, 48 + h:49 + h], in1=src[:],
                                op0=ALU.is_ge, op1=ALU.mult),
                                 reads=[sres, 'st5g'], writes=[f'cm{kc_}'])
                            for jb in range(4):
                                P.op('pe', lambda e, jb=jb, kc_=kc_, hi=hi: e.matmul(
                                    pacc[jb][:], lhsT=ident, rhs=cm[kc_][:, jb * 512:(jb + 1) * 512],
                                    start=(hi == 0), stop=(hi == 7)),
                                     reads=[f'cm{kc_}', 'cst'], writes=[f'pacc{jb}'])
                        for jb in range(4):
                            P.op('act', lambda e, jb=jb: e.activation(out=csum[:, jb * 512:(jb + 1) * 512], in_=pacc[jb][:],
                                                                      func=AF.Copy),
                                 reads=[f'pacc{jb}'], writes=[f'csum{jb}'])
                        for c in range(16):
                            P.op('pe', lambda e, c=c: e.transpose(out=pct[:, c * 128:(c + 1) * 128],
                                                                  in_=csum[:, c * 128:(c + 1) * 128], identity=ident),
                                 reads=[f'csum{c // 4}', 'cst'], writes=[f'pct{c // 8}'])
                        for b2 in range(2):
                            P.op('act', lambda e, b2=b2: e.activation(
                                out=ctb[cs_][:, b2 * 8:(b2 + 1) * 8, :].rearrange("p a n -> p (a n)"),
                                in_=pct[:, b2 * 1024:(b2 + 1) * 1024], func=AF.Copy),
                                 reads=[f'pct{b2}'], writes=[f'ctb{cs_}'])
                        P.dma(ct_scr[q8 * 16:(q8 + 1) * 16, :, blk * 128:(blk + 1) * 128].rearrange("a p n -> p a n"),
                              ctb[cs_][:], reads=[f'ctb{cs_}'], writes=['ct_scr'])
            P.wait_all_dma()
            P.barrier()
            P.flush()

        with ExitStack() as ph:
            def T(name, shape, dt):
                return ph.enter_context(nc.sbuf_tensor("p4_" + name, list(shape), dt))

            def PS(name, shape, dt):
                return ph.enter_context(nc.psum_tensor("ps4_" + name, list(shape), dt))

            G = 2
            oacc = T("oacc", [128, NBLK, D], F32)
            uf = [T(f"uf{i}", [128, G, D], F32) for i in range(2)]
            vf = [T(f"vf{i}", [128, G, D], F32) for i in range(2)]
            ub = T("ub", [128, G, D], BF16)
            vbb = [T(f"vbb{i}", [128, G, D], BF16) for i in range(4)]
            uT = [T(f"uT{i}", [128, G, 8, 128], BF16) for i in range(2)]
            ctg = [T(f"ctg{i}", [128, G, NBLK * 128], BF16) for i in range(2)]
            coefT = T("coefT", [128, 2 * G, NBLK * 128], BF16)
            gl = [T(f"gl{i}", [128, 512], F32) for i in range(2)]
            ptr = [PS(f"ptr{i}", [128, 1024], BF16) for i in range(2)]
            psa = [PS(f"psa{i}", [128, 512], F32) for i in range(2)]
            pso = [PS(f"pso{i}", [128, 512], F32) for i in range(2)]

            P.dma(oacc[:], h1_scr.rearrange("(s p) n -> p s n", p=128), reads=['h1_scr'], writes=['oacc'])
            NG = 128 // G
            for g in range(NG):
                b = g % 2
                vb4 = g % 4
                sg = g % 2
                e0 = g * G * 128
                P.dma(uf[b][:], peer_u[e0:e0 + G * 128, :].rearrange("(a p) n -> p a n", p=128), writes=[f'uf{b}'])
                P.dma(vf[b][:], peer_v[e0:e0 + G * 128, :].rearrange("(a p) n -> p a n", p=128), writes=[f'vf{b}'])
                P.dma(ctg[b][:], ct_scr[g * G:(g + 1) * G, :, :].rearrange("a p n -> p a n"), reads=['ct_scr'],
                      writes=[f'ctg{b}'])
                P.op('pool', lambda e, b=b: e.tensor_copy(out=ub[:], in_=uf[b][:]), reads=[f'uf{b}'], writes=['ub'])
                P.op('pool', lambda e, b=b, vb4=vb4: e.tensor_copy(out=vbb[vb4][:], in_=vf[b][:]), reads=[f'vf{b}'],
                     writes=[f'vbb{vb4}'])
                for a in range(G):
                    pb_ = a % 2
                    for c in range(8):
                        P.op('pe', lambda e, a=a, c=c, pb_=pb_: e.transpose(out=ptr[pb_][:, c * 128:(c + 1) * 128],
                                                                            in_=ub[:, a, c * 128:(c + 1) * 128],
                                                                            identity=ident),
                             reads=['ub', 'cst'], writes=[f'ptr{pb_}'])
                    P.op('dve', lambda e, a=a, b=b, pb_=pb_: e.tensor_tensor(
                        out=uT[b][:, a, :, :], in0=ptr[pb_][:].rearrange("p (c n) -> p c n", c=8),
                        in1=g2t.unsqueeze(2).to_broadcast([128, 8, 128]), op=ALU.mult),
                         reads=[f'ptr{pb_}', 'spk'], writes=[f'uT{b}_{a}'])
                for a in range(G):
                    for tg in range(4):
                        k = (a * 4 + tg) % 2
                        for c in range(8):
                            P.op('pe', lambda e, a=a, c=c, tg=tg, k=k, b=b: e.matmul(
                                psa[k][:], lhsT=uT[b][:, a, c, :], rhs=hn2T[:, c, tg * 512:(tg + 1) * 512],
                                start=(c == 0), stop=(c == 7)),
                                 reads=[f'uT{b}_{a}', 'hn2T'], writes=[f'psa{k}'])
                        P.op('act', lambda e, k=k: e.activation(out=gl[k][:], in_=psa[k][:], func=AF.Gelu),
                             reads=[f'psa{k}'], writes=[f'gl{k}'])
                        P.op('dve', lambda e, a=a, tg=tg, k=k, b=b: e.tensor_tensor(
                            out=coefT[:, sg * G + a, tg * 512:(tg + 1) * 512], in0=gl[k][:],
                            in1=ctg[b][:, a, tg * 512:(tg + 1) * 512], op=ALU.mult),
                             reads=[f'gl{k}', f'ctg{b}'], writes=[f'coefT{tg}'])
                if sg == 0:
                    continue
                for blk in range(NBLK):
                    for half in range(2):
                        k = (blk * 2 + half) % 2
                        for a4 in range(2 * G):
                            vsel = (g - 1 + a4 // G) % 4
                            P.op('pe', lambda e, a4=a4, blk=blk, half=half, k=k, vsel=vsel: e.matmul(
                                pso[k][:], lhsT=coefT[:, a4, blk * 128:(blk + 1) * 128],
                                rhs=vbb[vsel][:, a4 % G, half * 512:(half + 1) * 512],
                                start=(a4 == 0), stop=(a4 == 2 * G - 1)),
                                 reads=[f'coefT{blk // 4}', f'vbb{vsel}'], writes=[f'pso{k}'])
                        P.op('dve', lambda e, blk=blk, half=half, k=k: e.tensor_tensor(
                            out=oacc[:, blk, half * 512:(half + 1) * 512], in0=pso[k][:],
                            in1=oacc[:, blk, half * 512:(half + 1) * 512], op=ALU.add),
                             reads=[f'pso{k}', f'oacc{blk}_{half}', 'oacc'], writes=[f'oacc{blk}_{half}'])
            all_o = [f'oacc{blk}_{half}' for blk in range(NBLK) for half in range(2)]
            P.dma(out_d.rearrange("(s p) n -> p s n", p=128), oacc[:], reads=all_o + ['oacc'], writes=['out'])
            P.wait_all_dma()
            P.barrier()
            P.flush()
    return nc


def _host_prep(inputs):
    f32 = np.float32
    x = np.asarray(inputs["x"], f32)[0]
    meta = np.asarray(inputs["meta_tokens"], f32)
    hall = np.zeros((130 * 128, D), f32)
    hall[16:32] = meta
    hall[32:32 + SEQ] = x
    p = np.arange(128)
    cst = np.zeros((128, 640), f32)
    cst[:, 0:128] = np.eye(128)
    cst[:, 128:256] = -1.0 * (p[:, None] >= p[None, :])
    cst[:, 256:384] = -1.0 * (p[:, None] < p[None, :])
    cst[:, 384:512] = 1.0 / 512
    cst[:, 512:640] = -1.0
    cst = cst.astype(ml_dtypes.bfloat16)
    spk = np.zeros((128, C_END), f32)
    spk[:, C_G1:C_G1 + 8] = np.asarray(inputs["norm1_g"], f32)[0].reshape(8, 128).T
    spk[:, C_G2:C_G2 + 8] = np.asarray(inputs["norm2_g"], f32)[0].reshape(8, 128).T
    cw = np.asarray(inputs["conv_w"], f32)[0]
    spk[:, C_CW:C_CW + 124] = cw.reshape(31, 4, 128).transpose(2, 1, 0).reshape(128, 124)
    spk[:, C_CB:C_CB + 4] = np.asarray(inputs["conv_b"], f32)[0].reshape(4, 128).T
    spk[:, C_LG:C_LG + 4] = np.asarray(inputs["conv_ln_g"], f32)[0].reshape(4, 128).T
    spk[:, C_LB:C_LB + 4] = np.asarray(inputs["conv_ln_b"], f32)[0].reshape(4, 128).T
    spk[:, C_BG:C_BG + 16] = np.asarray(inputs["b_gate"], f32)[0].reshape(16, 128).T
    spk[:, C_GQ] = np.tile(np.asarray(inputs["q_norm_g"], f32)[0], 2)
    spk[:, C_GK] = np.tile(np.asarray(inputs["k_norm_g"], f32)[0], 2)
    peer_k = np.stack([np.asarray(inputs["peer_k1"], f32)[0], np.asarray(inputs["peer_k2"], f32)[0]], axis=1)
    peer_k = np.ascontiguousarray(peer_k.reshape(16, 128, 128))
    shared = {
        "hall": hall, "cst": cst, "spk": spk,
        "w_in": np.ascontiguousarray(np.asarray(inputs["w_in"], f32)[0]),
        "w_conv_out": np.ascontiguousarray(np.asarray(inputs["w_conv_out"], f32)[0]),
        "w_sb_out": np.ascontiguousarray(np.asarray(inputs["w_sb_out"], f32)[0]),
        "w_gate": np.ascontiguousarray(np.asarray(inputs["w_gate"], f32)[0]),
        "w_o": np.ascontiguousarray(np.asarray(inputs["w_o"], f32)[0]),
        "peer_wq": np.ascontiguousarray(np.asarray(inputs["peer_wq"], f32)[0]),
        "peer_k": peer_k,
        "peer_u": np.ascontiguousarray(np.asarray(inputs["peer_u"], f32)[0]),
        "peer_v": np.ascontiguousarray(np.asarray(inputs["peer_v"], f32)[0]),
    }
    in_maps = []
    kp = np.arange(128)[:, None]
    qn = np.arange(512)[None, :]
    for c in range(NCORE):
        m = dict(shared)
        xq = np.stack([hall[512 * (8 * j + c):512 * (8 * j + c) + SLOT + HALO] for j in range(NSLOT)])
        m["xq"] = np.ascontiguousarray(xq)
        am = np.stack([(128 * r + kp < 512 * c + 16 + qn) for r in range(33)], axis=1)
        m["amask"] = np.ascontiguousarray(am.reshape(128, 33 * 512).astype(f32).astype(ml_dtypes.bfloat16))
        in_maps.append(m)
    return in_maps


def _assemble(res):
    out = np.zeros((1, SEQ, D), np.float32)
    for c in range(NCORE):
        o = np.asarray(res.results[c]["out"], np.float32)
        for j in range(NSLOT):
            g = 8 * j + c
            out[0, 512 * g:512 * (g + 1)] = o[512 * j:512 * (j + 1)]
    return out


_NC_CACHE = {}


def kernel(**inputs):
    in_maps = _host_prep(inputs)
    if "nc" not in _NC_CACHE:
        _NC_CACHE["nc"] = build_program()
    res = run_bass_kernel_spmd(_NC_CACHE["nc"], in_maps, core_ids=list(range(NCORE)))
    return _assemble(res)
```
